# Optimizing a Trainium2 kernel written in Bass

```python
import math
import jax, jax.numpy as jnp
from jax import lax
import numpy as np

D_MODEL = 2048
BATCH = 8
SEQ = 4096
DEPTH = 2

H_A = 8
HD_A = 128
KV_RANK = 256
IDX_HEADS = 8
IDX_DIM = 64
TOPK_MAX = 256
H_B = 16
KVH_B = 2
GQA_GROUP = H_B // KVH_B
HD_B = 64
WINDOW = 128
BLOCK = 128
N_BUCKETS = 32
MAX_DISTANCE = 128
N_BIAS_HEADS = H_A + H_B
D_FF = 5632
EPS = 1e-6

W_QA = H_A * HD_A
W_CKV = KV_RANK
W_QI = IDX_HEADS * IDX_DIM
W_KI = IDX_DIM
W_WI = IDX_HEADS
W_QB = H_B * HD_B
W_KB = KVH_B * HD_B
W_VB = KVH_B * HD_B
D_IN = W_QA + W_CKV + W_QI + W_KI + W_WI + W_QB + W_KB + W_VB
D_MIX = H_A * HD_A + H_B * HD_B

kernel_name = "hymba_dsa_swa_macaron_t5"


def rmsnorm(x, g):
    xf = x.astype(jnp.float32)
    y = xf * lax.rsqrt(jnp.mean(xf * xf, axis=-1, keepdims=True) + EPS)
    return (y * g.astype(jnp.float32)).astype(x.dtype)


def layernorm(x, g, b):
    xf = x.astype(jnp.float32)
    mu = jnp.mean(xf, axis=-1, keepdims=True)
    var = jnp.mean(jnp.square(xf - mu), axis=-1, keepdims=True)
    y = (xf - mu) * lax.rsqrt(var + EPS)
    return (y * g.astype(jnp.float32) + b.astype(jnp.float32)).astype(x.dtype)


def swiglu(x, w_gate, w_up, w_down):
    return (jax.nn.silu(x @ w_gate) * (x @ w_up)) @ w_down


def t5_bucket(dist):
    n = jnp.maximum(dist, 0)
    max_exact = N_BUCKETS // 2
    nf = jnp.maximum(n, 1).astype(jnp.float32)
    large = max_exact + (jnp.log(nf / max_exact) / math.log(MAX_DISTANCE / max_exact)
                         * (N_BUCKETS - max_exact)).astype(jnp.int32)
    large = jnp.minimum(large, N_BUCKETS - 1)
    return jnp.where(n < max_exact, n, large)


def dsa_mixer(q, c_kv, q_idx, k_idx, w_idx, w_uk, w_uv, bias_table):
    b, s = q.shape[0], q.shape[1]
    nb = s // BLOCK
    k_sel = min(TOPK_MAX, s // 4)
    q_lat = jnp.einsum("bshd,rhd->bshr", q, w_uk)
    key_pos = jnp.arange(s)
    k_idx32 = k_idx.astype(jnp.float32)

    def to_blocks(a):
        return a.reshape((b, nb, BLOCK) + a.shape[2:]).swapaxes(0, 1)

    def block_fn(args):
        qi, ql, wi, blk = args
        t = blk * BLOCK + jnp.arange(BLOCK)
        dots = jnp.einsum("bthd,bsd->bths", qi.astype(jnp.float32), k_idx32)
        score = jnp.einsum("bth,bths->bts", wi.astype(jnp.float32), jax.nn.relu(dots))
        score = jnp.where((key_pos[None, :] <= t[:, None])[None], score, -jnp.inf)
        _, idx = lax.top_k(score, k_sel)
        c_sel = jax.vmap(lambda c, i: c[i])(c_kv, idx)
        dist = t[None, :, None] - idx
        bias = jnp.moveaxis(bias_table[t5_bucket(dist)], -1, 2).astype(jnp.float32)
        logits = jnp.einsum("bthr,btkr->bthk", ql, c_sel).astype(jnp.float32) * (HD_A ** -0.5) + bias
        logits = jnp.where((dist >= 0)[:, :, None, :], logits, -jnp.inf)
        p = jax.nn.softmax(logits, axis=-1).astype(c_sel.dtype)
        return jnp.einsum("bthk,btkr->bthr", p, c_sel)

    o_lat = lax.map(block_fn, (to_blocks(q_idx), to_blocks(q_lat), to_blocks(w_idx), jnp.arange(nb)))
    o_lat = o_lat.swapaxes(0, 1).reshape(b, s, H_A, KV_RANK)
    return jnp.einsum("bshr,rhd->bshd", o_lat, w_uv).reshape(b, s, H_A * HD_A)


def swa_mixer(q, k, v, sinks, bias_table):
    b, s = q.shape[0], q.shape[1]
    nb = s // BLOCK
    qb = q.reshape(b, nb, BLOCK, KVH_B, GQA_GROUP, HD_B).swapaxes(0, 1)

    def band(a):
        a = a.reshape(b, nb, BLOCK, KVH_B, HD_B)
        prev = jnp.concatenate([jnp.zeros_like(a[:, :1]), a[:, :-1]], axis=1)
        return jnp.concatenate([prev, a], axis=2).swapaxes(0, 1)

    kb, vb = band(k), band(v)
    qi_pos = jnp.arange(BLOCK)
    kj_pos = jnp.arange(2 * BLOCK)
    dist = BLOCK + qi_pos[:, None] - kj_pos[None, :]
    in_window = (dist >= 0) & (dist < WINDOW)
    bias = jnp.moveaxis(bias_table[t5_bucket(dist)], -1, 0)
    bias = bias.reshape(KVH_B, GQA_GROUP, BLOCK, 2 * BLOCK).astype(jnp.float32)
    sink = sinks.astype(jnp.float32).reshape(KVH_B, GQA_GROUP, 1, 1)

    def block_fn(args):
        qi, ki, vi, blk = args
        mask = in_window & (((blk - 1) * BLOCK + kj_pos) >= 0)[None, :]
        logits = jnp.einsum("btkgd,bskd->bkgts", qi, ki).astype(jnp.float32) * (HD_B ** -0.5) + bias
        logits = jnp.where(mask, logits, -jnp.inf)
        sink_col = jnp.broadcast_to(sink, logits.shape[:-1] + (1,))
        p = jax.nn.softmax(jnp.concatenate([logits, sink_col], axis=-1), axis=-1)[..., :-1]
        return jnp.einsum("bkgts,bskd->btkgd", p.astype(vi.dtype), vi)

    o = lax.map(block_fn, (qb, kb, vb, jnp.arange(nb)))
    return o.swapaxes(0, 1).reshape(b, s, H_B * HD_B)


def setup_inputs(seed: int = 0) -> dict:
    key = jax.random.key(seed)
    ks = jax.random.split(key, 24)
    f32 = jnp.float32

    def nrm(k, shape, scale):
        return jax.random.normal(k, shape, f32) * scale

    def gain(k, shape):
        return 1.0 + 0.01 * jax.random.normal(k, shape, f32)

    L = DEPTH
    return {
        "x": jax.random.normal(ks[0], (BATCH, SEQ, D_MODEL), f32),
        "rel_bias": nrm(ks[1], (N_BUCKETS, N_BIAS_HEADS), 0.3),
        "ffn1_norm": gain(ks[2], (L, D_MODEL)),
        "ffn1_gate": nrm(ks[3], (L, D_MODEL, D_FF), D_MODEL ** -0.5),
        "ffn1_up": nrm(ks[4], (L, D_MODEL, D_FF), D_MODEL ** -0.5),
        "ffn1_down": nrm(ks[5], (L, D_FF, D_MODEL), D_FF ** -0.5),
        "mix_norm": gain(ks[6], (L, D_MODEL)),
        "w_in": nrm(ks[7], (L, D_MODEL, D_IN), D_MODEL ** -0.5),
        "kv_norm": gain(ks[8], (L, KV_RANK)),
        "idx_k_norm_g": gain(ks[9], (L, IDX_DIM)),
        "idx_k_norm_b": nrm(ks[10], (L, IDX_DIM), 0.01),
        "w_uk": nrm(ks[11], (L, KV_RANK, H_A, HD_A), KV_RANK ** -0.5),
        "w_uv": nrm(ks[12], (L, KV_RANK, H_A, HD_A), KV_RANK ** -0.5),
        "sinks": nrm(ks[13], (L, H_B), 0.5),
        "w_out": nrm(ks[14], (L, D_MIX, D_MODEL), D_MIX ** -0.5),
        "ffn2_norm": gain(ks[15], (L, D_MODEL)),
        "ffn2_gate": nrm(ks[16], (L, D_MODEL, D_FF), D_MODEL ** -0.5),
        "ffn2_up": nrm(ks[17], (L, D_MODEL, D_FF), D_MODEL ** -0.5),
        "ffn2_down": nrm(ks[18], (L, D_FF, D_MODEL), D_FF ** -0.5),
        "final_norm": gain(ks[19], (D_MODEL,)),
    }


def reference(x, rel_bias, ffn1_norm, ffn1_gate, ffn1_up, ffn1_down, mix_norm, w_in, kv_norm,
              idx_k_norm_g, idx_k_norm_b, w_uk, w_uv, sinks, w_out, ffn2_norm, ffn2_gate,
              ffn2_up, ffn2_down, final_norm):
    b, s, _ = x.shape
    bias_a = rel_bias[:, :H_A]
    bias_b = rel_bias[:, H_A:]
    splits = [W_QA, W_CKV, W_QI, W_KI, W_WI, W_QB, W_KB]
    cuts = [int(c) for c in np.cumsum(splits)]
    h = x
    for l in range(DEPTH):
        h = h + 0.5 * swiglu(rmsnorm(h, ffn1_norm[l]), ffn1_gate[l], ffn1_up[l], ffn1_down[l])
        z = rmsnorm(h, mix_norm[l]) @ w_in[l]
        q_a, c_kv, q_i, k_i, w_i, q_b, k_b, v_b = jnp.split(z, cuts, axis=-1)
        q_a = q_a.reshape(b, s, H_A, HD_A)
        c_kv = rmsnorm(c_kv, kv_norm[l])
        q_i = q_i.reshape(b, s, IDX_HEADS, IDX_DIM)
        k_i = layernorm(k_i, idx_k_norm_g[l], idx_k_norm_b[l])
        w_i = w_i * (IDX_HEADS ** -0.5 * IDX_DIM ** -0.5)
        o_a = dsa_mixer(q_a, c_kv, q_i, k_i, w_i, w_uk[l], w_uv[l], bias_a)
        o_b = swa_mixer(q_b.reshape(b, s, H_B, HD_B), k_b.reshape(b, s, KVH_B, HD_B),
                        v_b.reshape(b, s, KVH_B, HD_B), sinks[l], bias_b)
        h = h + jnp.concatenate([o_a, o_b], axis=-1) @ w_out[l]
        h = h + 0.5 * swiglu(rmsnorm(h, ffn2_norm[l]), ffn2_gate[l], ffn2_up[l], ffn2_down[l])
    return rmsnorm(h, final_norm)
```

```python
import os
import math
from contextlib import ExitStack
import numpy as np
import concourse.bass as bass
import concourse.mybir as mybir
from concourse.bass_utils import run_bass_kernel_spmd

F32 = mybir.dt.float32
BF16 = mybir.dt.bfloat16
ALU = mybir.AluOpType
AF = mybir.ActivationFunctionType

D = 2048
S = 4096
L = 2
DFF = 5632
NFC = DFF // 128
NB = S // 128
EPS = 1e-6
NEG = -30000.0
NBIS = 16
BIS_LO = -16.0
BIS_W0 = 32.0
TOPK = 256


class Res:
    __slots__ = ("name", "w", "r", "dsem")

    def __init__(self, name):
        self.name = name
        self.w = None
        self.r = {}
        self.dsem = None


class Sem:
    def __init__(self, name, sem):
        self.name = name
        self.sem = sem
        self.cnt = 0


class Eng(Sem):
    def __init__(self, name, eng, sem, inorder=False):
        super().__init__(name, sem)
        self.eng = eng
        self.seen = {}
        self.inorder = inorder


class KB:
    def __init__(self, nlayers=L, dbg=None):
        self.nlayers = nlayers
        self.dbg = dbg
        self.nc = bass.Bass("TRN2", target_bir_lowering=False)
        self.es = ExitStack()
        nc = self.nc
        self.nsem = 0
        self.uid = 0
        self.stop_after = None
        self.pe = Eng("pe", nc.tensor, self._sem("pe"), inorder=True)
        self.act = Eng("act", nc.scalar, self._sem("act"))
        self.dve = Eng("dve", nc.vector, self._sem("dve"))
        self.sp = Eng("sp", nc.sync, self._sem("sp"))
        self.pool = Eng("pool", nc.gpsimd, self._sem("pool"))
        self.engs = [self.pe, self.act, self.dve, self.sp, self.pool]
        self.dsems = []

    def _sem(self, name):
        self.nsem += 1
        return self.es.enter_context(self.nc.semaphore(name))

    def sb(self, es, name, shape, dt):
        self.uid += 1
        name = f"{name}_{self.uid}"
        t = es.enter_context(self.nc.sbuf_tensor(name, shape, dt))
        return t, Res(name)

    def dsem_of(self, res):
        if res.dsem is None:
            res.dsem = Sem("d_" + res.name, self._sem("d_" + res.name))
            self.dsems.append(res.dsem)
        return res.dsem

    def _wait(self, E, reads, writes):
        deps = []
        for b in reads:
            if b.w is not None:
                deps.append(b.w)
        for b in writes:
            if b.w is not None:
                deps.append(b.w)
            deps.extend(b.r.values())
        for (X, c) in deps:
            if X is E and E.inorder:
                continue
            if E.seen.get(X.name, 0) >= c:
                continue
            E.eng.wait_ge(X.sem, c)
            E.seen[X.name] = c

    def _mark(self, tok, reads, writes):
        X = tok[0]
        for b in reads:
            b.r[X.name] = tok
        for b in writes:
            b.w = tok
            b.r = {}

    def I(self, E, fn, reads, writes, inc=True, **kw):
        self._wait(E, reads, writes)
        inst = getattr(E.eng, fn)(**kw)
        if inc:
            E.cnt += 1
            inst.then_inc(E.sem, 1)
            tok = (E, E.cnt)
        else:
            tok = (E, E.cnt + 1)
        self._mark(tok, reads, writes)
        return inst

    def dma(self, Q, out, in_, reads, writes, dres):
        ds = self.dsem_of(dres)
        self._wait(Q, reads, writes)
        inst = Q.eng.dma_start(out=out, in_=in_)
        ds.cnt += 16
        inst.then_inc(ds.sem, 16)
        self._mark((ds, ds.cnt), reads, writes)

    def barrier(self):
        allx = self.engs + self.dsems
        for E in self.engs:
            for X in allx:
                if X is E and E.inorder:
                    continue
                if X.cnt > E.seen.get(X.name, 0):
                    E.eng.wait_ge(X.sem, X.cnt)
                    E.seen[X.name] = X.cnt

    def mm(self, out, lhsT, rhs, start, stop, reads, writes, inc=None):
        if inc is None:
            inc = stop
        return self.I(self.pe, "matmul", reads, writes, inc=inc, out=out, lhsT=lhsT, rhs=rhs, start=start, stop=stop)

    def build(self):
        nc = self.nc
        es = self.es
        NL = self.nlayers
        dr = lambda name, shape, kind="ExternalInput": nc.dram_tensor(name, shape, F32, kind=kind).ap()
        self.xT = dr("xT", [D, S])
        self.gains = dr("gains", [128, (3 * L + 1) * 16])
        self.wg = dr("wg", [L * 2 * 44, 128, 16 * 128])
        self.wu = dr("wu", [L * 2 * 44, 128, 16 * 128])
        self.wd = dr("wd", [L * 2 * 16, 128, NFC * 128])
        self.winr = dr("winr", [L * 16, 128, 3272])
        self.wout = dr("wout", [L * 16, 128, 2048])
        self.c_identf = dr("c_identf", [128, 128])
        self.wuk = dr("wuk", [L, 128, 8 * 256])
        self.wuv = dr("wuv", [L, 128, 2 * 8 * 128])
        self.kvg = dr("kvg", [L, 256])
        self.kig = dr("kig", [L, 64])
        self.kib = dr("kib", [L, 64])
        self.sinkl = dr("sinkl", [L, 128, 8])
        self.relb = dr("relb", [32, 24])
        self.c_ident = dr("c_ident", [128, 128])
        self.c_caus = dr("c_caus", [128, 128])
        self.c_dm = dr("c_dm", [32, 32])
        self.c_ea = dr("c_ea", [33, 32768])
        self.c_eb = dr("c_eb", [33, 32768])
        self.outT = dr("outT", [D, S], kind="ExternalOutput")
        skind = "ExternalOutput" if self.dbg else "Internal"
        self.hA = dr("hA", [D, S], kind=skind)
        self.hB = dr("hB", [D, S], kind=skind)
        self.bsA = nc.dram_tensor("bsA", [8, 32768], BF16, kind="Internal").ap()
        self.bsB = nc.dram_tensor("bsB", [16, 32768], BF16, kind="Internal").ap()
        self.wscr = nc.dram_tensor("wscr", [L * 32, 128, 3272], BF16, kind="Internal").ap()

        self.ps = []
        for i in range(8):
            t = es.enter_context(nc.psum_tensor(f"ps{i}", [128, 512], F32))
            self.ps.append((t, Res(f"ps{i}")))
        self.ident, self.r_ident = self.sb(es, "ident", [128, 128], BF16)
        self.onesD, self.r_onesD = self.sb(es, "onesD", [128, 128], BF16)
        self.ones, self.r_ones = self.sb(es, "ones", [128, 128], BF16)
        self.g_sb, self.r_g = self.sb(es, "g_sb", [128, (3 * L + 1) * 16], F32)
        self.dma(self.pool, self.ident[:], self.c_ident[:, :], [], [self.r_ident], self.r_ident)
        self.dma(self.sp, self.g_sb[:], self.gains[:, :], [], [self.r_g], self.r_g)
        self.I(self.dve, "memset", [], [self.r_onesD], ap=self.onesD[:], constant=1.0 / D)
        self.I(self.dve, "memset", [], [self.r_ones], ap=self.ones[:], constant=1.0)

        self.setup_bias()
        self.barrier()

        seq = []
        for l in range(NL):
            seq.append(("ffn", l, 0))
            seq.append(("mix", l))
            seq.append(("ffn", l, 1))
        cur = self.xT
        bufs = [self.hA, self.hB]
        bi = 0
        if self.stop_after is not None:
            seq = seq[:self.stop_after]
        for i, ph in enumerate(seq):
            last = (i == len(seq) - 1) and self.stop_after is None
            dst = self.outT if last else bufs[bi]
            if ph[0] == "ffn":
                self.ffn_phase(cur, dst, ph[1], ph[2], final=last)
            else:
                self.mix_phase(cur, dst, ph[1])
            self.barrier()
            cur = dst
            bi ^= 1
        self.es.close()
        return nc

    def setup_bias(self):
        nc = self.nc
        with ExitStack() as es:
            tbl, r_tbl = self.sb(es, "tbl", [32, 24], F32)
            tblb, r_tblb = self.sb(es, "tblb", [32, 24], BF16)
            dm, r_dm = self.sb(es, "dm", [32, 32], BF16)
            lA, r_lA = self.sb(es, "lA", [33, 8], BF16)
            lB, r_lB = self.sb(es, "lB", [33, 16], BF16)
            E, r_E = self.sb(es, "Eoh", [33, 8192], BF16)
            E2, r_E2 = self.sb(es, "Eoh2", [33, 8192], BF16)
            ob, r_ob = self.sb(es, "obias", [16, 8192], BF16)
            ob2, r_ob2 = self.sb(es, "obias2", [16, 8192], BF16)
            self.dma(self.sp, tbl[:], self.relb[:, :], [], [r_tbl], r_tbl)
            self.dma(self.pool, dm[:], self.c_dm[:, :], [], [r_dm], r_dm)
            self.I(self.dve, "tensor_copy", [r_tbl], [r_tblb], out=tblb[:], in_=tbl[:])
            self.I(self.dve, "memset", [], [r_lA], ap=lA[:], constant=NEG)
            self.I(self.dve, "memset", [], [r_lB], ap=lB[:], constant=NEG)
            p0, r_p0 = self.ps[0]
            self.mm(p0[0:32, 0:8], dm[:], tblb[:, 0:8], True, True, [r_dm, r_tblb], [r_p0])
            self.I(self.dve, "tensor_copy", [r_p0], [r_lA], out=lA[0:32, :], in_=p0[0:32, 0:8])
            self.I(self.dve, "tensor_copy", [r_tblb], [r_lB], out=lB[0:32, :], in_=tblb[:, 8:24])
            for (src, lhs, r_lhs, nh, scr) in ((self.c_ea, lA, r_lA, 8, self.bsA), (self.c_eb, lB, r_lB, 16, self.bsB)):
                for q4 in range(4):
                    Eb, r_Eb = (E, r_E) if q4 % 2 == 0 else (E2, r_E2)
                    o_, r_o = (ob, r_ob) if q4 % 2 == 0 else (ob2, r_ob2)
                    self.dma(self.pool, Eb[:], src[:, q4 * 8192:(q4 + 1) * 8192], [], [r_Eb], r_Eb)
                    for j in range(16):
                        pb, r_pb = self.ps[1 + (j % 2)]
                        self.mm(pb[0:nh, :], lhs[:, 0:nh], Eb[:, j * 512:(j + 1) * 512], True, True, [r_lhs, r_Eb], [r_pb])
                        self.I(self.act, "activation", [r_pb], [r_o], out=o_[0:nh, j * 512:(j + 1) * 512], in_=pb[0:nh, :], func=AF.Copy)
                    self.dma(self.sp, scr[:, q4 * 8192:(q4 + 1) * 8192], o_[0:nh, :], [r_o], [], r_o)
            self.barrier()
    def load_bias(self, es):
        self.biasA, self.r_biasA = self.sb(es, "biasA", [128, 2, 1024], BF16)
        self.biasB, self.r_biasB = self.sb(es, "biasB", [128, 2, 2048], BF16)
        sA = self.bsA.rearrange("h (c j i) -> h c j i", c=2, j=128)
        for ci in range(2):
            for h in range(8):
                self.dma(self.sp, self.biasA[:, ci, h * 128:(h + 1) * 128], sA[h, ci, :, :], [], [self.r_biasA], self.r_biasA)
        sB = self.bsB.rearrange("h (c j i) -> h c j i", c=2, j=128)
        for ci in range(2):
            for k in range(2):
                for e in range(2):
                    for cp in range(4):
                        h = 8 * k + 2 * cp + e
                        off = ((k * 2 + e) * 4 + cp) * 128
                        self.dma(self.sp, self.biasB[:, ci, off:off + 128], sB[h, ci, :, :], [], [self.r_biasB], self.r_biasB)

    def norm(self, h, r_h, T, gcol, out, r_out, sq, rt, rstd, statbank):
        stat, r_stat = self.ps[statbank]
        for c in range(16):
            s_, r_s = sq[c % 2]
            self.I(self.act, "activation", [r_h], [r_s], out=s_[:, 0:T], in_=h[:, c, :], func=AF.Square)
            self.mm(stat[:, 0:T], self.onesD[:], s_[:, 0:T], c == 0, c == 15, [r_s, self.r_onesD], [r_stat], inc=True)
        rt_, r_rt = rt
        rs_, r_rs = rstd
        self.I(self.act, "activation", [r_stat, self.r_epsb], [r_rt], out=rt_[:, 0:T], in_=stat[:, 0:T], func=AF.Sqrt, bias=self.epsb[:, 0:1], scale=1.0)
        self.I(self.dve, "reciprocal", [r_rt], [r_rs], out=rs_[:, 0:T], in_=rt_[:, 0:T])
        for c in range(16):
            self.I(self.dve, "scalar_tensor_tensor", [r_h, r_rs, self.r_g], [r_out], out=out[:, c, :], in0=h[:, c, :],
                   scalar=self.g_sb[:, gcol * 16 + c:gcol * 16 + c + 1], in1=rs_[:, 0:T], op0=ALU.mult, op1=ALU.mult)

    def ffn_phase(self, src, dst, l, which, final):
        T = 512
        NT = S // T
        srcv = src.rearrange("(c p) t -> p c t", p=128)
        dstv = dst.rearrange("(c p) t -> p c t", p=128)
        fi = l * 2 + which
        gcol = l * 3 + (0 if which == 0 else 2)
        with ExitStack() as es:
            hb = [self.sb(es, f"f_h{i}", [128, 16, T], F32) for i in range(2)]
            hoist = not final
            xns = [self.sb(es, f"f_xn{i}", [128, 16, T], BF16) for i in range(2 if hoist else 1)]
            if hoist:
                sq16, r_sq16 = self.sb(es, "f_sq16", [128, 16, T], BF16)
            mid, r_mid = self.sb(es, "f_mid", [128, NFC, T], BF16)
            wgs = [self.sb(es, f"f_wg{i}", [128, 16, 128], BF16) for i in range(2)]
            wus = [self.sb(es, f"f_wu{i}", [128, 16, 128], BF16) for i in range(2)]
            wds = [self.sb(es, f"f_wd{i}", [128, NFC, 128], BF16) for i in range(2)]
            sq = [self.sb(es, f"f_sq{i}", [128, T], BF16) for i in range(2)]
            rt = self.sb(es, "f_rt", [128, T], F32)
            rstd = self.sb(es, "f_rstd", [128, T], F32)
            sg = [self.sb(es, f"f_sg{i}", [128, T], F32) for i in range(2)]
            self.epsb, self.r_epsb = self.sb(es, "f_eps", [128, 1], F32)
            self.I(self.dve, "memset", [], [self.r_epsb], ap=self.epsb[:], constant=EPS)

            units = []
            for t in range(NT):
                for u in range(44):
                    units.append(("gu", u))
                for o in range(16):
                    units.append(("d", o))
            state = {"next": 0}

            def prefetch(upto):
                while state["next"] <= upto and state["next"] < len(units):
                    kind, idx = units[state["next"]]
                    state["next"] += 1
                    slot = idx % 2
                    if kind == "gu":
                        w_, r_w = wgs[slot]
                        self.dma(self.pool, w_[:].rearrange("p c f -> p (c f)"), self.wg[fi * 44 + idx], [], [r_w], r_w)
                        w_, r_w = wus[slot]
                        self.dma(self.pool, w_[:].rearrange("p c f -> p (c f)"), self.wu[fi * 44 + idx], [], [r_w], r_w)
                    else:
                        w_, r_w = wds[slot]
                        self.dma(self.pool, w_[:].rearrange("p c f -> p (c f)"), self.wd[fi * 16 + idx], [], [r_w], r_w)

            h0, r_h0 = hb[0]
            self.dma(self.sp, h0[:], srcv[:, :, 0:T], [], [r_h0], r_h0)
            ui = 0
            prefetch(1)
            for t in range(NT):
                h, r_h = hb[t % 2]
                if t + 1 < NT:
                    hn, r_hn = hb[(t + 1) % 2]
                    self.dma(self.sp, hn[:], srcv[:, :, (t + 1) * T:(t + 2) * T], [], [r_hn], r_hn)
                xn, r_xn = xns[t % len(xns)]
                if t == 0 or not hoist:
                    self.norm(h, r_h, T, gcol, xn, r_xn, sq, rt, rstd, 0)
                for f in range(NFC):
                    prefetch(ui + 1)
                    ui += 1
                    wg_, r_wg = wgs[f % 2]
                    wu_, r_wu = wus[f % 2]
                    G, r_G = self.ps[1 + (f % 2)]
                    U, r_U = self.ps[3 + (f % 2)]
                    for c in range(16):
                        self.mm(G[:], wg_[:, c, :], xn[:, c, :], c == 0, c == 15, [r_wg, r_xn], [r_G])
                    for c in range(16):
                        self.mm(U[:], wu_[:, c, :], xn[:, c, :], c == 0, c == 15, [r_wu, r_xn], [r_U])
                    s_, r_s = sg[f % 2]
                    self.I(self.act, "activation", [r_G], [r_s], out=s_[:], in_=G[:], func=AF.Silu)
                    self.I(self.dve, "tensor_tensor", [r_s, r_U], [r_mid], out=mid[:, f, :], in0=s_[:], in1=U[:], op=ALU.mult)
                for o in range(16):
                    if hoist and t + 1 < NT:
                        hn, r_hn = hb[(t + 1) % 2]
                        xnn, r_xnn = xns[(t + 1) % 2]
                        if o == 0:
                            for c in range(16):
                                self.I(self.act, "activation", [r_hn], [r_sq16], out=sq16[:, c, :], in_=hn[:, c, :], func=AF.Square)
                        if o == 6:
                            stat, r_stat = self.ps[0]
                            for c in range(16):
                                self.mm(stat[:], self.onesD[:], sq16[:, c, :], c == 0, c == 15, [r_sq16, self.r_onesD], [r_stat])
                            rt_, r_rt = rt
                            rs_, r_rs = rstd
                            self.I(self.act, "activation", [r_stat, self.r_epsb], [r_rt], out=rt_[:], in_=stat[:], func=AF.Sqrt,
                                   bias=self.epsb[:, 0:1], scale=1.0)
                            self.I(self.dve, "reciprocal", [r_rt], [r_rs], out=rs_[:], in_=rt_[:])
                            for c in range(16):
                                self.I(self.dve, "scalar_tensor_tensor", [r_hn, r_rs, self.r_g], [r_xnn], out=xnn[:, c, :], in0=hn[:, c, :],
                                       scalar=self.g_sb[:, gcol * 16 + c:gcol * 16 + c + 1], in1=rs_[:], op0=ALU.mult, op1=ALU.mult)
                    prefetch(ui + 1)
                    ui += 1
                    wd_, r_wd = wds[o % 2]
                    O, r_O = self.ps[5 + (o % 2)]
                    for f in range(NFC):
                        self.mm(O[:], wd_[:, f, :], mid[:, f, :], f == 0, f == NFC - 1, [r_wd, r_mid], [r_O])
                    self.I(self.dve, "scalar_tensor_tensor", [r_O, r_h], [r_h], out=h[:, o, :], in0=O[:], scalar=0.5, in1=h[:, o, :],
                           op0=ALU.mult, op1=ALU.add)
                if final:
                    self.norm(h, r_h, T, 3 * L, h, r_h, sq, rt, rstd, 0)
                    self.dma(self.sp, dstv[:, :, t * T:(t + 1) * T], h[:], [r_h], [], r_h)
                else:
                    self.dma(self.sp, dstv[:, :, t * T:(t + 1) * T], h[:], [r_h], [], r_h)

    def mix_phase(self, src, dst, l):
        nc = self.nc
        srcv = src.rearrange("(c p) t -> p c t", p=128)
        dstv = dst.rearrange("(c p) t -> p c t", p=128)
        gcol = l * 3 + 1
        pe, act, dve, sp, pool = self.pe, self.act, self.dve, self.sp, self.pool
        I = self.I
        with ExitStack() as es:
            sb = lambda name, shape, dt: self.sb(es, name, shape, dt)
            self.load_bias(es)
            hb = [sb(f"m_h{i}", [128, 16, 128], F32) for i in range(3)]
            xn, r_xn = sb("m_xn", [128, 16, 128], BF16)
            sq16, r_sq16 = sb("m_sq16", [128, 16, 128], BF16)
            rt = sb("m_rt", [128, 128], F32)
            rstd = sb("m_rstd", [128, 128], F32)
            self.epsb, self.r_epsb = sb("m_eps", [128, 1], F32)
            I(dve, "memset", [], [self.r_epsb], ap=self.epsb[:], constant=EPS)
            wuk, r_wuk = sb("m_wuk", [128, 8, 256], BF16)
            wuv, r_wuv = sb("m_wuv", [128, 2, 8, 128], BF16)
            kvg, r_kvg = sb("m_kvg", [128, 256], F32)
            kig, r_kig = sb("m_kig", [128, 64], F32)
            kib, r_kib = sb("m_kib", [128, 64], F32)
            sink, r_sink = sb("m_sink", [128, 8], F32)
            esink, r_esink = sb("m_esink", [128, 8], F32)
            caus, r_caus = sb("m_caus", [128, 128], F32)
            self.dma(pool, wuk[:].rearrange("p c f -> p (c f)"), self.wuk[l], [], [r_wuk], r_wuk)
            self.dma(pool, wuv[:].rearrange("p a c f -> p (a c f)"), self.wuv[l], [], [r_wuv], r_wuv)
            self.dma(sp, kvg[:], self.kvg[l].partition_broadcast(128), [], [r_kvg], r_kvg)
            self.dma(sp, kig[:], self.kig[l].partition_broadcast(128), [], [r_kig], r_kig)
            self.dma(sp, kib[:], self.kib[l].partition_broadcast(128), [], [r_kib], r_kib)
            self.dma(sp, sink[:], self.sinkl[l], [], [r_sink], r_sink)
            self.dma(sp, caus[:], self.c_caus[:, :], [], [r_caus], r_caus)
            I(act, "activation", [r_sink], [r_esink], out=esink[:], in_=sink[:], func=AF.Exp)
            ring = [sb(f"m_ring{i}", [128, 3272], BF16) for i in range(3)]
            ztok, r_ztok = sb("m_ztok", [128, 2816], BF16)
            dtok, r_dtok = sb("m_dtok", [128, 2048], F32)
            identf, r_identf = sb("m_identf", [128, 128], F32)
            self.dma(sp, identf[:], self.c_identf[:, :], [], [r_identf], r_identf)
            ckv, _ = sb("m_ckv", [128, NB, 256], BF16)
            ckvT, _ = sb("m_ckvT", [128, 2, S], BF16)
            kiT, _ = sb("m_kiT", [128, S], BF16)
            r_ckv = [Res(f"ckv{i}") for i in range(NB)]
            r_ckvT = [Res(f"ckvT{i}") for i in range(NB)]
            r_kiT = [Res(f"kiT{i}") for i in range(NB)]
            kbT = [sb(f"m_kbT{i}", [128, 2, 128], BF16) for i in range(3)]
            vpad = [[[sb(f"m_vp{s}{k}{e}", [128, 128], BF16) for e in range(2)] for k in range(2)] for s in range(3)]
            opad = [sb(f"m_op{e}", [128, 128], BF16) for e in range(2)]
            for s_ in range(3):
                for k in range(2):
                    for e in range(2):
                        v_, r_v = vpad[s_][k][e]
                        I(dve, "memset", [], [r_v], ap=v_[:], constant=0.0)
            for e in range(2):
                o_, r_o = opad[e]
                I(dve, "memset", [], [r_o], ap=o_[:], constant=0.0)
                I(dve, "memset", [], [r_o], ap=o_[:, e * 64:(e + 1) * 64], constant=1.0)
            qaT, r_qaT = sb("m_qaT", [128, 8, 128], BF16)
            qiT, r_qiT = sb("m_qiT", [128, 4, 128], BF16)
            qbTs = [sb(f"m_qbT{i}", [128, 8, 128], BF16) for i in range(2)]
            qlatTs = [sb(f"m_qlatT{i}", [128, 2, 1024], BF16) for i in range(2)]
            olatT, r_olatT = sb("m_olatT", [128, 2, 1024], BF16)
            omix, r_omix = sb("m_omix", [128, 16, 128], BF16)
            junk, r_junk = sb("m_junk", [128, 256], F32)
            st1, r_st1 = sb("m_st1", [128, 16], F32)
            xc, r_xc = sb("m_xc", [128, 64], F32)
            kn, r_kn = sb("m_kn", [128, 64], F32)
            kiln, r_kiln = sb("m_kiln", [128, 128], BF16)
            wsc, r_wsc = sb("m_wsc", [128, 8], F32)
            diag, r_diag = sb("m_diag", [128, 8, 128], BF16)
            relu = [sb(f"m_relu{i}", [128, 512], BF16) for i in range(3)]
            scores, r_scores = sb("m_scores", [128, S], F32)
            mask, r_mask = sb("m_mask", [128, S], BF16)
            maskT, r_maskT = sb("m_maskT", [128, NB, 128], BF16)
            bs, r_bs = sb("m_bs", [128, 4], F32)
            eT = [sb(f"m_eT{i}", [128, 512], BF16) for i in range(3)]
            rss = [sb(f"m_rs{i}", [128, 512], F32) for i in range(1)]
            pB = [sb(f"m_pB{i}", [128, 512], BF16) for i in range(4)]
            dens = [sb(f"m_den{i}", [128, 512], F32) for i in range(1)]
            negb, r_negb = sb("m_negb", [128, 1], F32)
            I(dve, "memset", [], [r_negb], ap=negb[:], constant=NEG)

            ps = self.ps
            sched = [("in", 0, i) for i in range(16)]
            for qb in range(NB):
                if qb + 1 < NB:
                    sched += [("in", qb + 1, i) for i in range(16)]
                sched += [("out", qb, i) for i in range(16)]
            rstate = {"issued": 0}
            pending = []

            def ring_prefetch():
                while len(pending) < 2 and rstate["issued"] < len(sched):
                    n = rstate["issued"]
                    rstate["issued"] = n + 1
                    kind, _, i = sched[n]
                    w_, r_w = ring[n % 3]
                    if kind == "in":
                        self.dma(pool, w_[:], self.wscr[l * 32 + i], [], [r_w], r_w)
                    else:
                        self.dma(pool, w_[:, 0:2048], self.wscr[l * 32 + 16 + i][:, 0:2048], [], [r_w], r_w)
                    pending.append((w_, r_w))

            def ring_pop():
                w = pending.pop(0)
                ring_prefetch()
                return w

            for u in range(32):
                w_, r_w = ring[u % 3]
                if u < 16:
                    self.dma(pool, w_[:], self.winr[l * 16 + u], [], [r_w], r_w)
                    self.dma(sp, self.wscr[l * 32 + u], w_[:], [r_w], [], r_w)
                else:
                    self.dma(pool, w_[:, 0:2048], self.wout[l * 16 + u - 16], [], [r_w], r_w)
                    self.dma(sp, self.wscr[l * 32 + u][:, 0:2048], w_[:, 0:2048], [r_w], [], r_w)
            self.barrier()
            for i_ in range(3):
                h0, r_h0 = hb[i_]
                self.dma(sp, h0[:], srcv[:, :, i_ * 128:(i_ + 1) * 128], [], [r_h0], r_h0)
            ring_prefetch()

            def stageA1_sq(qb):
                h, r_h = hb[qb % 3]
                for c in range(16):
                    I(act, "activation", [r_h], [r_sq16], out=sq16[:, c, :], in_=h[:, c, :], func=AF.Square)

            def stageA1_rest(qb):
                h, r_h = hb[qb % 3]
                stat, r_stat = ps[7]
                for c in range(16):
                    self.mm(stat[:, 0:128], self.onesD[:], sq16[:, c, :], c == 0, c == 15, [r_sq16, self.r_onesD], [r_stat])
                rt_, r_rt = rt
                rs_, r_rs_ = rstd
                I(act, "activation", [r_stat, self.r_epsb], [r_rt], out=rt_[:], in_=stat[:, 0:128], func=AF.Sqrt, bias=self.epsb[:, 0:1], scale=1.0)
                I(dve, "reciprocal", [r_rt], [r_rs_], out=rs_[:], in_=rt_[:])
                for c in range(16):
                    I(dve, "scalar_tensor_tensor", [r_h, r_rs_, self.r_g], [r_xn], out=xn[:, c, :], in0=h[:, c, :],
                      scalar=self.g_sb[:, gcol * 16 + c:gcol * 16 + c + 1], in1=rs_[:], op0=ALU.mult, op1=ALU.mult)

            def stageA(qb):
                tok = slice(qb * 128, (qb + 1) * 128)
                slot3 = qb % 3
                TM, r_TM = ps[6]
                ncol = [512, 512, 512, 512, 512, 256]
                for c in range(16):
                    w_, r_w = ring_pop()
                    for g in range(6):
                        Z_, r_Z = ps[g]
                        self.mm(Z_[:, 0:ncol[g]], xn[:, c, :], w_[:, g * 512:g * 512 + ncol[g]], c == 0, c == 15, [r_xn, r_w], [r_Z])
                    self.mm(TM[:, 0:456], xn[:, c, :], w_[:, 2816:3272], c == 0, c == 15, [r_xn, r_w], [r_TM], inc=True)
                for g in range(6):
                    Z_, r_Z = ps[g]
                    sc = float(64 ** -0.5) if g in (3, 4) else 1.0
                    if g % 2 == 0:
                        I(act, "activation", [r_Z], [r_ztok], out=ztok[:, g * 512:g * 512 + ncol[g]], in_=Z_[:, 0:ncol[g]], func=AF.Copy, scale=sc)
                    else:
                        I(dve, "tensor_scalar", [r_Z], [r_ztok], out=ztok[:, g * 512:g * 512 + ncol[g]], in0=Z_[:, 0:ncol[g]], scalar1=sc, scalar2=None,
                          op0=ALU.mult)
                I(act, "activation", [r_TM], [r_junk, r_st1], out=junk[:, 0:256], in_=TM[:, 0:256], func=AF.Square, accum_out=st1[:, 0:1])
                I(act, "activation", [r_st1, self.r_epsb], [r_st1], out=st1[:, 1:2], in_=st1[:, 0:1], func=AF.Sqrt, bias=self.epsb[:, 0:1], scale=1.0 / 256)
                I(dve, "reciprocal", [r_st1], [r_st1], out=st1[:, 2:3], in_=st1[:, 1:2])
                I(dve, "scalar_tensor_tensor", [r_TM, r_st1, r_kvg], [r_ckv[qb]], out=ckv[:, qb, :], in0=TM[:, 0:256], scalar=st1[:, 2:3],
                  in1=kvg[:], op0=ALU.mult, op1=ALU.mult)
                I(act, "activation", [r_TM], [r_junk, r_st1], out=junk[:, 0:64], in_=TM[:, 256:320], func=AF.Copy, accum_out=st1[:, 3:4])
                I(dve, "tensor_scalar", [r_st1], [r_st1], out=st1[:, 4:5], in0=st1[:, 3:4], scalar1=-1.0 / 64, scalar2=None, op0=ALU.mult)
                I(dve, "tensor_scalar", [r_TM, r_st1], [r_xc], out=xc[:], in0=TM[:, 256:320], scalar1=st1[:, 4:5], scalar2=None, op0=ALU.add)
                I(act, "activation", [r_xc], [r_junk, r_st1], out=junk[:, 0:64], in_=xc[:], func=AF.Square, accum_out=st1[:, 5:6])
                I(act, "activation", [r_st1, self.r_epsb], [r_st1], out=st1[:, 6:7], in_=st1[:, 5:6], func=AF.Sqrt, bias=self.epsb[:, 0:1], scale=1.0 / 64)
                I(dve, "reciprocal", [r_st1], [r_st1], out=st1[:, 7:8], in_=st1[:, 6:7])
                I(dve, "scalar_tensor_tensor", [r_xc, r_st1, r_kig], [r_kn], out=kn[:], in0=xc[:], scalar=st1[:, 7:8], in1=kig[:],
                  op0=ALU.mult, op1=ALU.mult)
                I(dve, "tensor_tensor", [r_kn, r_kib], [r_kiln], out=kiln[:, 0:64], in0=kn[:], in1=kib[:], op=ALU.add)
                I(dve, "tensor_tensor", [r_kn, r_kib], [r_kiln], out=kiln[:, 64:128], in0=kn[:], in1=kib[:], op=ALU.add)
                I(dve, "tensor_scalar", [r_TM], [r_wsc], out=wsc[:], in0=TM[:, 320:328], scalar1=float(512 ** -0.5), scalar2=None, op0=ALU.mult)
                for hh in range(8):
                    I(dve, "tensor_scalar", [r_wsc, self.r_ident], [r_diag], out=diag[:, hh, :], in0=self.ident[:], scalar1=wsc[:, hh:hh + 1],
                      scalar2=None, op0=ALU.mult)
                for k in range(2):
                    for e in range(2):
                        v_, r_v = vpad[slot3][k][e]
                        I(act, "activation", [r_TM], [r_v], out=v_[:, e * 64:(e + 1) * 64], in_=TM[:, 328 + k * 64:328 + (k + 1) * 64], func=AF.Copy)
                qbT, r_qbT = qbTs[qb % 2]
                kb_, r_kb = kbT[slot3]

                def tgroup(chunks, bank, outs):
                    B_, r_B = ps[bank]
                    Bb = B_[:].bitcast(BF16)
                    n = len(chunks)
                    for j, ch in enumerate(chunks):
                        I(pe, "transpose", [r_ztok, self.r_ident], [r_B], out=Bb[:, j * 128:(j + 1) * 128], in_=ztok[:, ch * 128:(ch + 1) * 128],
                          identity=self.ident[:], inc=(j == n - 1))
                    for (eng, dst_ap, r_dst, j0, nj) in outs:
                        src_ap = Bb[:, j0 * 128:(j0 + nj) * 128].rearrange("p (g t) -> p g t", g=nj)
                        if eng is act:
                            I(act, "activation", [r_B], [r_dst], out=dst_ap, in_=src_ap, func=AF.Copy)
                        else:
                            I(dve, "tensor_copy", [r_B], [r_dst], out=dst_ap, in_=src_ap)

                tgroup(list(range(0, 8)), 0, [(act, qaT[:, :, :], r_qaT, 0, 8)])
                tgroup(list(range(8, 12)) + [20, 21], 1, [(dve, qiT[:, :, :], r_qiT, 0, 4), (dve, kb_[:, :, :], r_kb, 4, 2)])
                tgroup(list(range(12, 20)), 2, [(act, qbT[:, :, :], r_qbT, 0, 8)])
                TP, r_TP = ps[5]
                TPb = TP[:].bitcast(BF16)
                for rc in range(2):
                    I(pe, "transpose", [r_ckv[qb], self.r_ident], [r_TP], out=TPb[:, rc * 128:(rc + 1) * 128], in_=ckv[:, qb, rc * 128:(rc + 1) * 128],
                      identity=self.ident[:], inc=False)
                I(pe, "transpose", [r_kiln, self.r_ident], [r_TP], out=TPb[:, 256:384], in_=kiln[:], identity=self.ident[:])
                for rc in range(2):
                    I(act, "activation", [r_TP], [r_ckvT[qb]], out=ckvT[:, rc, tok], in_=TPb[:, rc * 128:(rc + 1) * 128], func=AF.Copy)
                I(act, "activation", [r_TP], [r_kiT[qb]], out=kiT[:, tok], in_=TPb[:, 256:384], func=AF.Copy)

                qlatT, r_qlatT = qlatTs[qb % 2]
                for b4 in range(4):
                    rc = b4 // 2
                    Q_, r_Q = ps[1 + b4]
                    for hi in range(4):
                        hh = (b4 % 2) * 4 + hi
                        self.mm(Q_[:, hi * 128:(hi + 1) * 128], wuk[:, hh, rc * 128:(rc + 1) * 128], qaT[:, hh, :], True, True, [r_wuk, r_qaT], [r_Q],
                                inc=(hi == 3))
                    I(act, "activation", [r_Q], [r_qlatT], out=qlatT[:, rc, (b4 % 2) * 512:(b4 % 2 + 1) * 512], in_=Q_[:], func=AF.Copy,
                      scale=float(128 ** -0.5))

            def stageI(qb):
                if qb < 2:
                    return
                nk = (qb + 1) * 128
                nkg = (nk + 511) // 512
                items = [(kg, hh) for kg in range(nkg) for hh in range(8)]
                scb = [ps[4], ps[7]]

                def emitD(i):
                    kg, hh = items[i]
                    ncols = min(512, nk - kg * 512)
                    kres = [r_kiT[b_] for b_ in range(kg * 4, min(kg * 4 + 4, qb + 1))]
                    c, e = hh // 2, hh % 2
                    Dk, r_Dk = ps[5 + (i % 2)]
                    self.mm(Dk[:, 0:ncols], qiT[e * 64:(e + 1) * 64, c, :], kiT[e * 64:(e + 1) * 64, kg * 512:kg * 512 + ncols], True, True,
                            [r_qiT] + kres, [r_Dk])

                emitD(0)
                for i, (kg, hh) in enumerate(items):
                    if i + 1 < len(items):
                        emitD(i + 1)
                    ncols = min(512, nk - kg * 512)
                    Dk, r_Dk = ps[5 + (i % 2)]
                    rl, r_rl = relu[i % 3]
                    I(act, "activation", [r_Dk], [r_rl], out=rl[:, 0:ncols], in_=Dk[:, 0:ncols], func=AF.Relu)
                    SC, r_SC = scb[kg % 2]
                    self.mm(SC[:, 0:ncols], diag[:, hh, :], rl[:, 0:ncols], hh == 0, hh == 7, [r_diag, r_rl], [r_SC])
                    if hh == 7:
                        if kg == nkg - 1:
                            if ncols > 128:
                                I(dve, "tensor_copy", [r_SC], [r_scores], out=scores[:, kg * 512:kg * 512 + ncols - 128], in_=SC[:, 0:ncols - 128])
                            I(dve, "tensor_tensor", [r_SC, r_caus], [r_scores], out=scores[:, nk - 128:nk], in0=SC[:, ncols - 128:ncols], in1=caus[:],
                              op=ALU.add)
                        else:
                            I(dve, "tensor_copy", [r_SC], [r_scores], out=scores[:, kg * 512:(kg + 1) * 512], in_=SC[:, 0:512])

            def stageB(qb):
                if qb < 2:
                    return
                nk = (qb + 1) * 128
                I(dve, "memset", [], [r_bs], ap=bs[:, 0:1], constant=BIS_LO + BIS_W0 / 2)
                for it in range(NBIS):
                    wk = BIS_W0 / (2 ** (it + 1))
                    wn = BIS_W0 / (2 ** (it + 2))
                    I(dve, "tensor_scalar", [r_scores, r_bs], [r_mask, r_bs], out=mask[:, 0:nk], in0=scores[:, 0:nk], scalar1=bs[:, 0:1],
                      scalar2=None, op0=ALU.is_ge, op1=ALU.add, accum_out=bs[:, 1:2])
                    I(dve, "tensor_scalar", [r_bs], [r_bs], out=bs[:, 2:3], in0=bs[:, 1:2], scalar1=TOPK - 0.5, scalar2=wk, op0=ALU.is_ge,
                      op1=ALU.mult)
                    addc = (wn - wk) if it < NBIS - 1 else (-wk)
                    I(dve, "scalar_tensor_tensor", [r_bs], [r_bs], out=bs[:, 0:1], in0=bs[:, 0:1], scalar=addc, in1=bs[:, 2:3], op0=ALU.add,
                      op1=ALU.add)
                    yield
                I(dve, "tensor_scalar", [r_scores, r_bs], [r_mask], out=mask[:, 0:nk], in0=scores[:, 0:nk], scalar1=bs[:, 0:1], scalar2=None,
                  op0=ALU.is_ge)
                yield

            def stageBT(qb):
                if qb < 2:
                    return
                for g in range((qb + 1 + 3) // 4):
                    MT, r_MT = ps[5 + (g % 2)]
                    MTb = MT[:].bitcast(BF16)
                    n = min(4, qb + 1 - g * 4)
                    for j in range(n):
                        kc = g * 4 + j
                        I(pe, "transpose", [r_mask, self.r_ident], [r_MT], out=MTb[:, j * 128:(j + 1) * 128], in_=mask[:, kc * 128:(kc + 1) * 128],
                          identity=self.ident[:], inc=(j == n - 1))
                    I(act, "activation", [r_MT, r_negb], [r_maskT], out=maskT[:, g * 4:g * 4 + n, :],
                      in_=MTb[:, 0:n * 128].rearrange("p (g t) -> p g t", g=n), func=AF.Identity, scale=-NEG, bias=negb[:, 0:1])

            def stageC(qb, bgen):
                def bstep():
                    if bgen is not None:
                        next(bgen, None)

                tok = slice(qb * 128, (qb + 1) * 128)
                h, r_h = hb[qb % 3]
                use_mask = qb >= 2
                if qb + 2 < NB:
                    stageA1_sq(qb + 2)
                qlatT, r_qlatT = qlatTs[qb % 2]
                qbT, r_qbT = qbTs[qb % 2]
                steps = [(hf, kc) for hf in range(2) for kc in range(qb + 1)]
                accb = [(ps[2], ps[3], ps[4]), (ps[5], ps[6], ps[7])]

                def emitST(i):
                    hf, kc = steps[i]
                    hs = slice(hf * 512, (hf + 1) * 512)
                    ST, r_ST = ps[i % 2]
                    ks = slice(kc * 128, (kc + 1) * 128)
                    hasb = kc >= qb - 1
                    self.mm(ST[:], ckvT[:, 0, ks], qlatT[:, 0, hs], True, False, [r_ckvT[kc], r_qlatT], [r_ST])
                    self.mm(ST[:], ckvT[:, 1, ks], qlatT[:, 1, hs], False, not (hasb or use_mask), [r_ckvT[kc], r_qlatT], [r_ST])
                    if hasb:
                        bi_ = 1 if kc == qb else 0
                        self.mm(ST[:], self.ident[:], self.biasA[:, bi_, hs], False, not use_mask, [self.r_ident, self.r_biasA], [r_ST])
                    if use_mask:
                        self.mm(ST[:].rearrange("p (g t) -> p g t", g=4), self.ident[:],
                                maskT[:, kc, :].unsqueeze(1).to_broadcast([128, 4, 128]), False, True, [self.r_ident, r_maskT], [r_ST])

                emitST(0)
                for i, (hf, kc) in enumerate(steps):
                    if i + 1 < len(steps):
                        emitST(i + 1)
                    hs = slice(hf * 512, (hf + 1) * 512)
                    (ACC0, r_A0), (ACC1, r_A1), (SUMS, r_SU) = accb[hf]
                    ST, r_ST = ps[i % 2]
                    p_, r_p = eT[i % 3]
                    I(act, "activation", [r_ST], [r_p], out=p_[:], in_=ST[:], func=AF.Exp)
                    first, lastk = kc == 0, kc == qb
                    self.mm(ACC0[:], ckv[:, kc, 0:128], p_[:], first, lastk, [r_ckv[kc], r_p], [r_A0], inc=False)
                    self.mm(ACC1[:], ckv[:, kc, 128:256], p_[:], first, lastk, [r_ckv[kc], r_p], [r_A1], inc=False)
                    self.mm(SUMS[:], self.ones[:], p_[:], first, lastk, [self.r_ones, r_p], [r_SU], inc=True)
                    bstep()
                    bstep()
                    if lastk:
                        rs, r_rs = rss[0]
                        I(dve, "reciprocal", [r_SU], [r_rs], out=rs[:], in_=SUMS[:])
                        I(dve, "tensor_tensor", [r_A0, r_rs], [r_olatT], out=olatT[:, 0, hs], in0=ACC0[:], in1=rs[:], op=ALU.mult)
                        I(dve, "tensor_tensor", [r_A1, r_rs], [r_olatT], out=olatT[:, 1, hs], in0=ACC1[:], in1=rs[:], op=ALU.mult)
                chunks = [(qb - 1, 0), (qb, 1)] if qb > 0 else [(qb, 1)]
                sw = [(k, kcb, ci, e) for k in range(2) for (kcb, ci) in chunks for e in range(2)]

                def emitS2(i):
                    k, kcb, ci, e = sw[i]
                    kb2, r_kb2 = kbT[kcb % 3]
                    arr = 0 if k == e else 1
                    ST2, r_ST2 = ps[2 + (i % 2)]
                    self.mm(ST2[:].rearrange("p (g t) -> p g t", g=4), kb2[e * 64:(e + 1) * 64, arr, :], qbT[e * 64:(e + 1) * 64, 4 * k:4 * k + 4, :],
                            True, False, [r_kb2, r_qbT], [r_ST2])
                    off = (k * 2 + e) * 512
                    self.mm(ST2[:], self.ident[:], self.biasB[:, ci, off:off + 512], False, True, [self.r_ident, self.r_biasB], [r_ST2])

                emitS2(0)
                pbs = []
                for i, (k, kcb, ci, e) in enumerate(sw):
                    if i + 1 < len(sw):
                        emitS2(i + 1)
                    ST2, r_ST2 = ps[2 + (i % 2)]
                    pb_, r_pb = pB[i % 4]
                    I(act, "activation", [r_ST2], [r_pb], out=pb_[:], in_=ST2[:], func=AF.Exp)
                    pbs.append((pb_, r_pb, kcb % 3, e))
                    if i + 1 < len(sw) and sw[i + 1][0] == k:
                        continue
                    OB, r_OB = ps[4]
                    SB, r_SB = ps[k]
                    npb = len(pbs)
                    for cp in range(4):
                        for i_, (pb2, r_pb2, kslot, e2) in enumerate(pbs):
                            v_, r_v = vpad[kslot][k][e2]
                            self.mm(OB[:, cp * 128:(cp + 1) * 128], v_[:], pb2[:, cp * 128:(cp + 1) * 128], i_ == 0, i_ == npb - 1, [r_v, r_pb2], [r_OB],
                                    inc=False)
                        for i_, (pb2, r_pb2, kslot, e2) in enumerate(pbs):
                            o_, r_o = opad[e2]
                            self.mm(SB[:, cp * 128:(cp + 1) * 128], o_[:], pb2[:, cp * 128:(cp + 1) * 128], i_ == 0, i_ == npb - 1, [r_o, r_pb2], [r_SB],
                                    inc=(cp == 3 and i_ == npb - 1))
                    den, r_den = dens[0]
                    for cp in range(4):
                        c = 4 * k + cp
                        I(dve, "tensor_scalar", [r_SB, r_esink], [r_den], out=den[:, cp * 128:(cp + 1) * 128], in0=SB[:, cp * 128:(cp + 1) * 128],
                          scalar1=esink[:, c:c + 1], scalar2=None, op0=ALU.add)
                    I(dve, "reciprocal", [r_den], [r_den], out=den[:], in_=den[:])
                    I(dve, "tensor_tensor", [r_OB, r_den], [r_omix], out=omix[:, 8 + 4 * k:8 + 4 * k + 4, :],
                      in0=OB[:].rearrange("p (g t) -> p g t", g=4), in1=den[:].rearrange("p (g t) -> p g t", g=4), op=ALU.mult)
                    pbs = []
                    bstep()
                if bgen is not None:
                    for _ in bgen:
                        pass
                if qb + 1 < NB:
                    stageBT(qb + 1)
                if qb + 2 < NB:
                    stageA1_rest(qb + 2)
                for g in range(2):
                    OA, r_OA = ps[5 + g]
                    for hi in range(4):
                        hh = g * 4 + hi
                        for rc in range(2):
                            self.mm(OA[:, hi * 128:(hi + 1) * 128], wuv[:, rc, hh, :], olatT[:, rc, hh * 128:(hh + 1) * 128], rc == 0, rc == 1,
                                    [r_wuv, r_olatT], [r_OA], inc=(hi == 3 and rc == 1))
                    I(act, "activation", [r_OA], [r_omix], out=omix[:, g * 4:(g + 1) * 4, :], in_=OA[:].rearrange("p (g t) -> p g t", g=4), func=AF.Copy)
                for c in range(16):
                    w_, r_w = ring_pop()
                    for g in range(4):
                        WO, r_WO = ps[g]
                        self.mm(WO[:], omix[:, c, :], w_[:, g * 512:(g + 1) * 512], c == 0, c == 15, [r_omix, r_w], [r_WO], inc=(g == 3 or c == 15))
                for g in range(4):
                    WO, r_WO = ps[g]
                    if g % 2 == 0:
                        I(act, "activation", [r_WO], [r_dtok], out=dtok[:, g * 512:(g + 1) * 512], in_=WO[:], func=AF.Copy)
                    else:
                        I(dve, "tensor_copy", [r_WO], [r_dtok], out=dtok[:, g * 512:(g + 1) * 512], in_=WO[:])
                bstep()
                for g in range(4):
                    TR, r_TR = ps[4 + g]
                    for oi in range(4):
                        o = g * 4 + oi
                        I(pe, "transpose", [r_dtok, r_identf], [r_TR], out=TR[:, oi * 128:(oi + 1) * 128], in_=dtok[:, o * 128:(o + 1) * 128],
                          identity=identf[:], inc=(oi == 3))
                    I(dve, "tensor_tensor", [r_TR, r_h], [r_h], out=h[:, g * 4:(g + 1) * 4, :], in0=TR[:].rearrange("p (g t) -> p g t", g=4),
                      in1=h[:, g * 4:(g + 1) * 4, :], op=ALU.add)
                    bstep()
                self.dma(sp, dstv[:, :, tok], h[:], [r_h], [], r_h)
                if qb + 3 < NB:
                    self.dma(sp, h[:], srcv[:, :, (qb + 3) * 128:(qb + 4) * 128], [], [r_h], r_h)
                if bgen is not None:
                    for _ in bgen:
                        pass

            stageA1_sq(0)
            stageA1_rest(0)
            stageA(0)
            stageI(0)
            stageA1_sq(1)
            stageA1_rest(1)
            for qb in range(NB):
                if qb + 1 < NB:
                    stageA(qb + 1)
                    stageI(qb + 1)
                bgen = stageB(qb + 1) if qb + 1 < NB else None
                stageC(qb, bgen)


def _t5_bucket(dist):
    n = np.maximum(dist, 0)
    nf = np.maximum(n, 1).astype(np.float32)
    large = 16 + (np.log(nf / np.float32(16)) / np.float32(math.log(128 / 16)) * np.float32(16)).astype(np.int32)
    large = np.minimum(large, 31)
    return np.where(n < 16, n, large)


def _consts():
    ident = np.eye(128, dtype=np.float32)
    q = np.arange(128)[:, None]
    j = np.arange(128)[None, :]
    caus = np.where(j > q, NEG, 0.0).astype(np.float32)
    dm = np.eye(32, dtype=np.float32)
    dm[31, :] -= 1.0
    jj = np.arange(128)[:, None]
    ii = np.arange(128)[None, :]
    ea = np.zeros((33, 2, 128, 128), np.float32)
    eb = np.zeros((33, 2, 128, 128), np.float32)
    for ci in range(2):
        dist = (128 + ii - jj) if ci == 0 else (ii - jj)
        bk = _t5_bucket(dist)
        va = dist >= 0
        vb = (dist >= 0) & (dist < 128)
        for b in range(32):
            ea[b, ci] = ((bk == b) & va)
            eb[b, ci] = ((bk == b) & vb)
        ea[32, ci] = ~va
        eb[32, ci] = ~vb
    return ident, caus, dm, ea.reshape(33, -1), eb.reshape(33, -1)


_COLS_FM = (list(range(0, 1024)) + list(range(1280, 1792)) + list(range(1864, 2888)) + list(range(2888, 3016))
            + list(range(2952, 3016)) + list(range(2888, 2952)))
_COLS_TM = list(range(1024, 1280)) + list(range(1792, 1856)) + list(range(1856, 1864)) + list(range(3016, 3144))


def _prep_shared(inp):
    f = lambda a: np.ascontiguousarray(a, dtype=np.float32)
    gl = []
    for l in range(L):
        gl += [inp["ffn1_norm"][l], inp["mix_norm"][l], inp["ffn2_norm"][l]]
    gl.append(inp["final_norm"])
    gains = np.concatenate([np.asarray(g).reshape(16, 128).T for g in gl], axis=1)
    wg, wu, wd = [], [], []
    for l in range(L):
        for (g, u, d_) in ((inp["ffn1_gate"], inp["ffn1_up"], inp["ffn1_down"]), (inp["ffn2_gate"], inp["ffn2_up"], inp["ffn2_down"])):
            wg.append(np.asarray(g[l]).reshape(16, 128, 44, 128).transpose(2, 1, 0, 3).reshape(44, 128, 2048))
            wu.append(np.asarray(u[l]).reshape(16, 128, 44, 128).transpose(2, 1, 0, 3).reshape(44, 128, 2048))
            wd.append(np.asarray(d_[l]).reshape(NFC, 128, 16, 128).transpose(2, 1, 0, 3).reshape(16, 128, NFC * 128))
    winfm, wintm, wout, wuk, wuv, sinkl = [], [], [], [], [], []
    for l in range(L):
        w = np.asarray(inp["w_in"][l])
        winfm.append(w[:, _COLS_FM + _COLS_TM].reshape(16, 128, 3272))
        wout.append(np.asarray(inp["w_out"][l]).reshape(16, 128, 2048))
        wuk.append(np.asarray(inp["w_uk"][l]).transpose(2, 1, 0).reshape(128, 2048))
        wuv.append(np.asarray(inp["w_uv"][l]).reshape(2, 128, 8, 128).transpose(1, 0, 2, 3).reshape(128, 2048))
        s = np.asarray(inp["sinks"][l])
        sl = np.zeros((128, 8), np.float32)
        for c in range(8):
            sl[0:64, c] = s[2 * c]
            sl[64:128, c] = s[2 * c + 1]
        sinkl.append(sl)
    ident, caus, dm, ea, eb = _consts()
    return {
        "gains": f(gains), "wg": f(np.concatenate(wg, 0)), "wu": f(np.concatenate(wu, 0)), "wd": f(np.concatenate(wd, 0)),
        "winr": f(np.concatenate(winfm, 0)), "wout": f(np.concatenate(wout, 0)), "c_identf": f(ident),
        "wuk": f(np.stack(wuk, 0)), "wuv": f(np.stack(wuv, 0)), "kvg": f(inp["kv_norm"]), "kig": f(inp["idx_k_norm_g"]),
        "kib": f(inp["idx_k_norm_b"]), "sinkl": f(np.stack(sinkl, 0)), "relb": f(inp["rel_bias"]),
        "c_ident": f(ident), "c_caus": f(caus), "c_dm": f(dm), "c_ea": f(ea), "c_eb": f(eb),
    }


def kernel(**inputs):
    x = np.asarray(inputs["x"], dtype=np.float32)
    B = x.shape[0]
    shared = _prep_shared(inputs)
    kb = KB()
    nc = kb.build()
    in_maps = []
    for b in range(B):
        m = dict(shared)
        m["xT"] = np.ascontiguousarray(x[b].T)
        in_maps.append(m)
    res = run_bass_kernel_spmd(nc, in_maps, core_ids=list(range(B)))
    out = np.stack([np.ascontiguousarray(r["outT"].T) for r in res.results], axis=0)
    return out.astype(np.float32)
```

```python
import os
import math
from contextlib import ExitStack
import numpy as np
import concourse.bass as bass
import concourse.mybir as mybir
from concourse.bass_utils import run_bass_kernel_spmd

F32 = mybir.dt.float32
BF16 = mybir.dt.bfloat16
ALU = mybir.AluOpType
AF = mybir.ActivationFunctionType

D = 2048
S = 4096
L = 2
DFF = 5632
NFC = DFF // 128
NB = S // 128
EPS = 1e-6
NEG = -30000.0
NBIS = 16
BIS_LO = -16.0
BIS_W0 = 32.0
TOPK = 256


class Res:
    __slots__ = ("name", "w", "r", "dsem")

    def __init__(self, name):
        self.name = name
        self.w = None
        self.r = {}
        self.dsem = None


class Sem:
    def __init__(self, name, sem):
        self.name = name
        self.sem = sem
        self.cnt = 0


class Eng(Sem):
    def __init__(self, name, eng, sem, inorder=False):
        super().__init__(name, sem)
        self.eng = eng
        self.seen = {}
        self.inorder = inorder


class KB:
    def __init__(self, nlayers=L, dbg=None):
        self.nlayers = nlayers
        self.dbg = dbg
        self.nc = bass.Bass("TRN2", target_bir_lowering=False)
        self.es = ExitStack()
        nc = self.nc
        self.nsem = 0
        self.uid = 0
        self.stop_after = None
        self.pe = Eng("pe", nc.tensor, self._sem("pe"), inorder=True)
        self.act = Eng("act", nc.scalar, self._sem("act"))
        self.dve = Eng("dve", nc.vector, self._sem("dve"))
        self.sp = Eng("sp", nc.sync, self._sem("sp"))
        self.pool = Eng("pool", nc.gpsimd, self._sem("pool"))
        self.engs = [self.pe, self.act, self.dve, self.sp, self.pool]
        self.dsems = []

    def _sem(self, name):
        self.nsem += 1
        return self.es.enter_context(self.nc.semaphore(name))

    def sb(self, es, name, shape, dt):
        self.uid += 1
        name = f"{name}_{self.uid}"
        t = es.enter_context(self.nc.sbuf_tensor(name, shape, dt))
        return t, Res(name)

    def dsem_of(self, res):
        if res.dsem is None:
            res.dsem = Sem("d_" + res.name, self._sem("d_" + res.name))
            self.dsems.append(res.dsem)
        return res.dsem

    def _wait(self, E, reads, writes):
        deps = []
        for b in reads:
            if b.w is not None:
                deps.append(b.w)
        for b in writes:
            if b.w is not None:
                deps.append(b.w)
            deps.extend(b.r.values())
        for (X, c) in deps:
            if X is E and E.inorder:
                continue
            if E.seen.get(X.name, 0) >= c:
                continue
            E.eng.wait_ge(X.sem, c)
            E.seen[X.name] = c

    def _mark(self, tok, reads, writes):
        X = tok[0]
        for b in reads:
            b.r[X.name] = tok
        for b in writes:
            b.w = tok
            b.r = {}

    def I(self, E, fn, reads, writes, inc=True, **kw):
        self._wait(E, reads, writes)
        inst = getattr(E.eng, fn)(**kw)
        if inc:
            E.cnt += 1
            inst.then_inc(E.sem, 1)
            tok = (E, E.cnt)
        else:
            tok = (E, E.cnt + 1)
        self._mark(tok, reads, writes)
        return inst

    def dma(self, Q, out, in_, reads, writes, dres):
        ds = self.dsem_of(dres)
        self._wait(Q, reads, writes)
        inst = Q.eng.dma_start(out=out, in_=in_)
        ds.cnt += 16
        inst.then_inc(ds.sem, 16)
        self._mark((ds, ds.cnt), reads, writes)

    def barrier(self):
        allx = self.engs + self.dsems
        for E in self.engs:
            for X in allx:
                if X is E and E.inorder:
                    continue
                if X.cnt > E.seen.get(X.name, 0):
                    E.eng.wait_ge(X.sem, X.cnt)
                    E.seen[X.name] = X.cnt

    def mm(self, out, lhsT, rhs, start, stop, reads, writes, inc=None):
        if inc is None:
            inc = stop
        return self.I(self.pe, "matmul", reads, writes, inc=inc, out=out, lhsT=lhsT, rhs=rhs, start=start, stop=stop)

    def build(self):
        nc = self.nc
        es = self.es
        NL = self.nlayers
        dr = lambda name, shape, kind="ExternalInput": nc.dram_tensor(name, shape, F32, kind=kind).ap()
        self.xT = dr("xT", [D, S])
        self.gains = dr("gains", [128, (3 * L + 1) * 16])
        self.wg = dr("wg", [L * 2 * 44, 128, 16 * 128])
        self.wu = dr("wu", [L * 2 * 44, 128, 16 * 128])
        self.wd = dr("wd", [L * 2 * 16, 128, NFC * 128])
        self.winr = dr("winr", [L * 16, 128, 3272])
        self.wout = dr("wout", [L * 16, 128, 2048])
        self.c_identf = dr("c_identf", [128, 128])
        self.wuk = dr("wuk", [L, 128, 8 * 256])
        self.wuv = dr("wuv", [L, 128, 2 * 8 * 128])
        self.kvg = dr("kvg", [L, 256])
        self.kig = dr("kig", [L, 64])
        self.kib = dr("kib", [L, 64])
        self.sinkl = dr("sinkl", [L, 128, 8])
        self.relb = dr("relb", [32, 24])
        self.c_ident = dr("c_ident", [128, 128])
        self.c_caus = dr("c_caus", [128, 128])
        self.c_dm = dr("c_dm", [32, 32])
        self.c_ea = dr("c_ea", [33, 32768])
        self.c_eb = dr("c_eb", [33, 32768])
        self.outT = dr("outT", [D, S], kind="ExternalOutput")
        skind = "ExternalOutput" if self.dbg else "Internal"
        self.hA = dr("hA", [D, S], kind=skind)
        self.hB = dr("hB", [D, S], kind=skind)
        self.bsA = nc.dram_tensor("bsA", [8, 32768], BF16, kind="Internal").ap()
        self.bsB = nc.dram_tensor("bsB", [16, 32768], BF16, kind="Internal").ap()
        self.wscr = nc.dram_tensor("wscr", [L * 32, 128, 3272], BF16, kind="Internal").ap()

        self.ps = []
        for i in range(8):
            t = es.enter_context(nc.psum_tensor(f"ps{i}", [128, 512], F32))
            self.ps.append((t, Res(f"ps{i}")))
        self.ident, self.r_ident = self.sb(es, "ident", [128, 128], BF16)
        self.onesD, self.r_onesD = self.sb(es, "onesD", [128, 128], BF16)
        self.ones, self.r_ones = self.sb(es, "ones", [128, 128], BF16)
        self.g_sb, self.r_g = self.sb(es, "g_sb", [128, (3 * L + 1) * 16], F32)
        self.dma(self.pool, self.ident[:], self.c_ident[:, :], [], [self.r_ident], self.r_ident)
        self.dma(self.sp, self.g_sb[:], self.gains[:, :], [], [self.r_g], self.r_g)
        self.I(self.dve, "memset", [], [self.r_onesD], ap=self.onesD[:], constant=1.0 / D)
        self.I(self.dve, "memset", [], [self.r_ones], ap=self.ones[:], constant=1.0)

        self.setup_bias()
        self.barrier()

        seq = []
        for l in range(NL):
            seq.append(("ffn", l, 0))
            seq.append(("mix", l))
            seq.append(("ffn", l, 1))
        cur = self.xT
        bufs = [self.hA, self.hB]
        bi = 0
        if self.stop_after is not None:
            seq = seq[:self.stop_after]
        for i, ph in enumerate(seq):
            last = (i == len(seq) - 1) and self.stop_after is None
            dst = self.outT if last else bufs[bi]
            if ph[0] == "ffn":
                self.ffn_phase(cur, dst, ph[1], ph[2], final=last)
            else:
                self.mix_phase(cur, dst, ph[1])
            self.barrier()
            cur = dst
            bi ^= 1
        self.es.close()
        return nc

    def setup_bias(self):
        nc = self.nc
        with ExitStack() as es:
            tbl, r_tbl = self.sb(es, "tbl", [32, 24], F32)
            tblb, r_tblb = self.sb(es, "tblb", [32, 24], BF16)
            dm, r_dm = self.sb(es, "dm", [32, 32], BF16)
            lA, r_lA = self.sb(es, "lA", [33, 8], BF16)
            lB, r_lB = self.sb(es, "lB", [33, 16], BF16)
            E, r_E = self.sb(es, "Eoh", [33, 8192], BF16)
            E2, r_E2 = self.sb(es, "Eoh2", [33, 8192], BF16)
            ob, r_ob = self.sb(es, "obias", [16, 8192], BF16)
            ob2, r_ob2 = self.sb(es, "obias2", [16, 8192], BF16)
            self.dma(self.sp, tbl[:], self.relb[:, :], [], [r_tbl], r_tbl)
            self.dma(self.pool, dm[:], self.c_dm[:, :], [], [r_dm], r_dm)
            self.I(self.dve, "tensor_copy", [r_tbl], [r_tblb], out=tblb[:], in_=tbl[:])
            self.I(self.dve, "memset", [], [r_lA], ap=lA[:], constant=NEG)
            self.I(self.dve, "memset", [], [r_lB], ap=lB[:], constant=NEG)
            p0, r_p0 = self.ps[0]
            self.mm(p0[0:32, 0:8], dm[:], tblb[:, 0:8], True, True, [r_dm, r_tblb], [r_p0])
            self.I(self.dve, "tensor_copy", [r_p0], [r_lA], out=lA[0:32, :], in_=p0[0:32, 0:8])
            self.I(self.dve, "tensor_copy", [r_tblb], [r_lB], out=lB[0:32, :], in_=tblb[:, 8:24])
            for (src, lhs, r_lhs, nh, scr) in ((self.c_ea, lA, r_lA, 8, self.bsA), (self.c_eb, lB, r_lB, 16, self.bsB)):
                for q4 in range(4):
                    Eb, r_Eb = (E, r_E) if q4 % 2 == 0 else (E2, r_E2)
                    o_, r_o = (ob, r_ob) if q4 % 2 == 0 else (ob2, r_ob2)
                    self.dma(self.pool, Eb[:], src[:, q4 * 8192:(q4 + 1) * 8192], [], [r_Eb], r_Eb)
                    for j in range(16):
                        pb, r_pb = self.ps[1 + (j % 2)]
                        self.mm(pb[0:nh, :], lhs[:, 0:nh], Eb[:, j * 512:(j + 1) * 512], True, True, [r_lhs, r_Eb], [r_pb])
                        self.I(self.act, "activation", [r_pb], [r_o], out=o_[0:nh, j * 512:(j + 1) * 512], in_=pb[0:nh, :], func=AF.Copy)
                    self.dma(self.sp, scr[:, q4 * 8192:(q4 + 1) * 8192], o_[0:nh, :], [r_o], [], r_o)
            self.barrier()
    def load_bias(self, es):
        self.biasA, self.r_biasA = self.sb(es, "biasA", [128, 2, 1024], BF16)
        self.biasB, self.r_biasB = self.sb(es, "biasB", [128, 2, 2048], BF16)
        sA = self.bsA.rearrange("h (c j i) -> h c j i", c=2, j=128)
        for ci in range(2):
            for h in range(8):
                self.dma(self.sp, self.biasA[:, ci, h * 128:(h + 1) * 128], sA[h, ci, :, :], [], [self.r_biasA], self.r_biasA)
        sB = self.bsB.rearrange("h (c j i) -> h c j i", c=2, j=128)
        for ci in range(2):
            for k in range(2):
                for e in range(2):
                    for cp in range(4):
                        h = 8 * k + 2 * cp + e
                        off = ((k * 2 + e) * 4 + cp) * 128
                        self.dma(self.sp, self.biasB[:, ci, off:off + 128], sB[h, ci, :, :], [], [self.r_biasB], self.r_biasB)

    def norm(self, h, r_h, T, gcol, out, r_out, sq, rt, rstd, statbank):
        stat, r_stat = self.ps[statbank]
        for c in range(16):
            s_, r_s = sq[c % 2]
            self.I(self.act, "activation", [r_h], [r_s], out=s_[:, 0:T], in_=h[:, c, :], func=AF.Square)
            self.mm(stat[:, 0:T], self.onesD[:], s_[:, 0:T], c == 0, c == 15, [r_s, self.r_onesD], [r_stat], inc=True)
        rt_, r_rt = rt
        rs_, r_rs = rstd
        self.I(self.act, "activation", [r_stat, self.r_epsb], [r_rt], out=rt_[:, 0:T], in_=stat[:, 0:T], func=AF.Sqrt, bias=self.epsb[:, 0:1], scale=1.0)
        self.I(self.dve, "reciprocal", [r_rt], [r_rs], out=rs_[:, 0:T], in_=rt_[:, 0:T])
        for c in range(16):
            self.I(self.dve, "scalar_tensor_tensor", [r_h, r_rs, self.r_g], [r_out], out=out[:, c, :], in0=h[:, c, :],
                   scalar=self.g_sb[:, gcol * 16 + c:gcol * 16 + c + 1], in1=rs_[:, 0:T], op0=ALU.mult, op1=ALU.mult)

    def ffn_phase(self, src, dst, l, which, final):
        T = 512
        NT = S // T
        srcv = src.rearrange("(c p) t -> p c t", p=128)
        dstv = dst.rearrange("(c p) t -> p c t", p=128)
        fi = l * 2 + which
        gcol = l * 3 + (0 if which == 0 else 2)
        with ExitStack() as es:
            hb = [self.sb(es, f"f_h{i}", [128, 16, T], F32) for i in range(2)]
            hoist = not final
            xns = [self.sb(es, f"f_xn{i}", [128, 16, T], BF16) for i in range(2 if hoist else 1)]
            if hoist:
                sq16, r_sq16 = self.sb(es, "f_sq16", [128, 16, T], BF16)
            mid, r_mid = self.sb(es, "f_mid", [128, NFC, T], BF16)
            wgs = [self.sb(es, f"f_wg{i}", [128, 16, 128], BF16) for i in range(2)]
            wus = [self.sb(es, f"f_wu{i}", [128, 16, 128], BF16) for i in range(2)]
            wds = [self.sb(es, f"f_wd{i}", [128, NFC, 128], BF16) for i in range(2)]
            sq = [self.sb(es, f"f_sq{i}", [128, T], BF16) for i in range(2)]
            rt = self.sb(es, "f_rt", [128, T], F32)
            rstd = self.sb(es, "f_rstd", [128, T], F32)
            sg = [self.sb(es, f"f_sg{i}", [128, T], F32) for i in range(2)]
            self.epsb, self.r_epsb = self.sb(es, "f_eps", [128, 1], F32)
            self.I(self.dve, "memset", [], [self.r_epsb], ap=self.epsb[:], constant=EPS)

            units = []
            for t in range(NT):
                for u in range(44):
                    units.append(("gu", u))
                for o in range(16):
                    units.append(("d", o))
            state = {"next": 0}

            def prefetch(upto):
                while state["next"] <= upto and state["next"] < len(units):
                    kind, idx = units[state["next"]]
                    state["next"] += 1
                    slot = idx % 2
                    if kind == "gu":
                        w_, r_w = wgs[slot]
                        self.dma(self.pool, w_[:].rearrange("p c f -> p (c f)"), self.wg[fi * 44 + idx], [], [r_w], r_w)
                        w_, r_w = wus[slot]
                        self.dma(self.pool, w_[:].rearrange("p c f -> p (c f)"), self.wu[fi * 44 + idx], [], [r_w], r_w)
                    else:
                        w_, r_w = wds[slot]
                        self.dma(self.pool, w_[:].rearrange("p c f -> p (c f)"), self.wd[fi * 16 + idx], [], [r_w], r_w)

            h0, r_h0 = hb[0]
            self.dma(self.sp, h0[:], srcv[:, :, 0:T], [], [r_h0], r_h0)
            ui = 0
            prefetch(1)
            for t in range(NT):
                h, r_h = hb[t % 2]
                if t + 1 < NT:
                    hn, r_hn = hb[(t + 1) % 2]
                    self.dma(self.sp, hn[:], srcv[:, :, (t + 1) * T:(t + 2) * T], [], [r_hn], r_hn)
                xn, r_xn = xns[t % len(xns)]
                if t == 0 or not hoist:
                    self.norm(h, r_h, T, gcol, xn, r_xn, sq, rt, rstd, 0)
                for f in range(NFC):
                    prefetch(ui + 1)
                    ui += 1
                    wg_, r_wg = wgs[f % 2]
                    wu_, r_wu = wus[f % 2]
                    G, r_G = self.ps[1 + (f % 2)]
                    U, r_U = self.ps[3 + (f % 2)]
                    for c in range(16):
                        self.mm(G[:], wg_[:, c, :], xn[:, c, :], c == 0, c == 15, [r_wg, r_xn], [r_G])
                    for c in range(16):
                        self.mm(U[:], wu_[:, c, :], xn[:, c, :], c == 0, c == 15, [r_wu, r_xn], [r_U])
                    s_, r_s = sg[f % 2]
                    self.I(self.act, "activation", [r_G], [r_s], out=s_[:], in_=G[:], func=AF.Silu)
                    self.I(self.dve, "tensor_tensor", [r_s, r_U], [r_mid], out=mid[:, f, :], in0=s_[:], in1=U[:], op=ALU.mult)
                for o in range(16):
                    if hoist and t + 1 < NT:
                        hn, r_hn = hb[(t + 1) % 2]
                        xnn, r_xnn = xns[(t + 1) % 2]
                        if o == 0:
                            for c in range(16):
                                self.I(self.act, "activation", [r_hn], [r_sq16], out=sq16[:, c, :], in_=hn[:, c, :], func=AF.Square)
                        if o == 6:
                            stat, r_stat = self.ps[0]
                            for c in range(16):
                                self.mm(stat[:], self.onesD[:], sq16[:, c, :], c == 0, c == 15, [r_sq16, self.r_onesD], [r_stat])
                            rt_, r_rt = rt
                            rs_, r_rs = rstd
                            self.I(self.act, "activation", [r_stat, self.r_epsb], [r_rt], out=rt_[:], in_=stat[:], func=AF.Sqrt,
                                   bias=self.epsb[:, 0:1], scale=1.0)
                            self.I(self.dve, "reciprocal", [r_rt], [r_rs], out=rs_[:], in_=rt_[:])
                            for c in range(16):
                                self.I(self.dve, "scalar_tensor_tensor", [r_hn, r_rs, self.r_g], [r_xnn], out=xnn[:, c, :], in0=hn[:, c, :],
                                       scalar=self.g_sb[:, gcol * 16 + c:gcol * 16 + c + 1], in1=rs_[:], op0=ALU.mult, op1=ALU.mult)
                    prefetch(ui + 1)
                    ui += 1
                    wd_, r_wd = wds[o % 2]
                    O, r_O = self.ps[5 + (o % 2)]
                    for f in range(NFC):
                        self.mm(O[:], wd_[:, f, :], mid[:, f, :], f == 0, f == NFC - 1, [r_wd, r_mid], [r_O])
                    self.I(self.dve, "scalar_tensor_tensor", [r_O, r_h], [r_h], out=h[:, o, :], in0=O[:], scalar=0.5, in1=h[:, o, :],
                           op0=ALU.mult, op1=ALU.add)
                if final:
                    self.norm(h, r_h, T, 3 * L, h, r_h, sq, rt, rstd, 0)
                    self.dma(self.sp, dstv[:, :, t * T:(t + 1) * T], h[:], [r_h], [], r_h)
                else:
                    self.dma(self.sp, dstv[:, :, t * T:(t + 1) * T], h[:], [r_h], [], r_h)

    def mix_phase(self, src, dst, l):
        nc = self.nc
        srcv = src.rearrange("(c p) t -> p c t", p=128)
        dstv = dst.rearrange("(c p) t -> p c t", p=128)
        gcol = l * 3 + 1
        pe, act, dve, sp, pool = self.pe, self.act, self.dve, self.sp, self.pool
        I = self.I
        with ExitStack() as es:
            sb = lambda name, shape, dt: self.sb(es, name, shape, dt)
            self.load_bias(es)
            hb = [sb(f"m_h{i}", [128, 16, 128], F32) for i in range(3)]
            xn, r_xn = sb("m_xn", [128, 16, 128], BF16)
            sq16, r_sq16 = sb("m_sq16", [128, 16, 128], BF16)
            rt = sb("m_rt", [128, 128], F32)
            rstd = sb("m_rstd", [128, 128], F32)
            self.epsb, self.r_epsb = sb("m_eps", [128, 1], F32)
            I(dve, "memset", [], [self.r_epsb], ap=self.epsb[:], constant=EPS)
            wuk, r_wuk = sb("m_wuk", [128, 8, 256], BF16)
            wuv, r_wuv = sb("m_wuv", [128, 2, 8, 128], BF16)
            kvg, r_kvg = sb("m_kvg", [128, 256], F32)
            kig, r_kig = sb("m_kig", [128, 64], F32)
            kib, r_kib = sb("m_kib", [128, 64], F32)
            sink, r_sink = sb("m_sink", [128, 8], F32)
            esink, r_esink = sb("m_esink", [128, 8], F32)
            caus, r_caus = sb("m_caus", [128, 128], F32)
            self.dma(pool, wuk[:].rearrange("p c f -> p (c f)"), self.wuk[l], [], [r_wuk], r_wuk)
            self.dma(pool, wuv[:].rearrange("p a c f -> p (a c f)"), self.wuv[l], [], [r_wuv], r_wuv)
            self.dma(sp, kvg[:], self.kvg[l].partition_broadcast(128), [], [r_kvg], r_kvg)
            self.dma(sp, kig[:], self.kig[l].partition_broadcast(128), [], [r_kig], r_kig)
            self.dma(sp, kib[:], self.kib[l].partition_broadcast(128), [], [r_kib], r_kib)
            self.dma(sp, sink[:], self.sinkl[l], [], [r_sink], r_sink)
            self.dma(sp, caus[:], self.c_caus[:, :], [], [r_caus], r_caus)
            I(act, "activation", [r_sink], [r_esink], out=esink[:], in_=sink[:], func=AF.Exp)
            ring = [sb(f"m_ring{i}", [128, 3272], BF16) for i in range(3)]
            ztok, r_ztok = sb("m_ztok", [128, 2816], BF16)
            dtok, r_dtok = sb("m_dtok", [128, 2048], F32)
            identf, r_identf = sb("m_identf", [128, 128], F32)
            self.dma(sp, identf[:], self.c_identf[:, :], [], [r_identf], r_identf)
            ckv, _ = sb("m_ckv", [128, NB, 256], BF16)
            ckvT, _ = sb("m_ckvT", [128, 2, S], BF16)
            kiT, _ = sb("m_kiT", [128, S], BF16)
            r_ckv = [Res(f"ckv{i}") for i in range(NB)]
            r_ckvT = [Res(f"ckvT{i}") for i in range(NB)]
            r_kiT = [Res(f"kiT{i}") for i in range(NB)]
            kbT = [sb(f"m_kbT{i}", [128, 2, 128], BF16) for i in range(3)]
            vpad = [[[sb(f"m_vp{s}{k}{e}", [128, 128], BF16) for e in range(2)] for k in range(2)] for s in range(3)]
            opad = [sb(f"m_op{e}", [128, 128], BF16) for e in range(2)]
            for s_ in range(3):
                for k in range(2):
                    for e in range(2):
                        v_, r_v = vpad[s_][k][e]
                        I(dve, "memset", [], [r_v], ap=v_[:], constant=0.0)
            for e in range(2):
                o_, r_o = opad[e]
                I(dve, "memset", [], [r_o], ap=o_[:], constant=0.0)
                I(dve, "memset", [], [r_o], ap=o_[:, e * 64:(e + 1) * 64], constant=1.0)
            qaT, r_qaT = sb("m_qaT", [128, 8, 128], BF16)
            qiT, r_qiT = sb("m_qiT", [128, 4, 128], BF16)
            qbTs = [sb(f"m_qbT{i}", [128, 8, 128], BF16) for i in range(2)]
            qlatTs = [sb(f"m_qlatT{i}", [128, 2, 1024], BF16) for i in range(2)]
            olatT, r_olatT = sb("m_olatT", [128, 2, 1024], BF16)
            omix, r_omix = sb("m_omix", [128, 16, 128], BF16)
            junk, r_junk = sb("m_junk", [128, 256], F32)
            st1, r_st1 = sb("m_st1", [128, 16], F32)
            xc, r_xc = sb("m_xc", [128, 64], F32)
            kn, r_kn = sb("m_kn", [128, 64], F32)
            kiln, r_kiln = sb("m_kiln", [128, 128], BF16)
            wsc, r_wsc = sb("m_wsc", [128, 8], F32)
            diag, r_diag = sb("m_diag", [128, 8, 128], BF16)
            relu = [sb(f"m_relu{i}", [128, 512], BF16) for i in range(3)]
            scores, r_scores = sb("m_scores", [128, S], F32)
            mask, r_mask = sb("m_mask", [128, S], BF16)
            maskT, r_maskT = sb("m_maskT", [128, NB, 128], BF16)
            bs, r_bs = sb("m_bs", [128, 4], F32)
            eT = [sb(f"m_eT{i}", [128, 512], BF16) for i in range(3)]
            rss = [sb(f"m_rs{i}", [128, 512], F32) for i in range(1)]
            pB = [sb(f"m_pB{i}", [128, 512], BF16) for i in range(4)]
            dens = [sb(f"m_den{i}", [128, 512], F32) for i in range(1)]
            negb, r_negb = sb("m_negb", [128, 1], F32)
            I(dve, "memset", [], [r_negb], ap=negb[:], constant=NEG)

            ps = self.ps
            sched = [("in", 0, i) for i in range(16)]
            for qb in range(NB):
                if qb + 1 < NB:
                    sched += [("in", qb + 1, i) for i in range(16)]
                sched += [("out", qb, i) for i in range(16)]
            rstate = {"issued": 0}
            pending = []

            def ring_prefetch():
                while len(pending) < 2 and rstate["issued"] < len(sched):
                    n = rstate["issued"]
                    rstate["issued"] = n + 1
                    kind, _, i = sched[n]
                    w_, r_w = ring[n % 3]
                    if kind == "in":
                        self.dma(pool, w_[:], self.wscr[l * 32 + i], [], [r_w], r_w)
                    else:
                        self.dma(pool, w_[:, 0:2048], self.wscr[l * 32 + 16 + i][:, 0:2048], [], [r_w], r_w)
                    pending.append((w_, r_w))

            def ring_pop():
                w = pending.pop(0)
                ring_prefetch()
                return w

            r_wst = [Res(f"m_wst{i}_{l}") for i in range(3)]
            for u in range(32):
                w_, r_w = ring[u % 3]
                if u < 16:
                    self.dma(pool, w_[:], self.winr[l * 16 + u], [], [r_w], r_w)
                    self.dma(sp, self.wscr[l * 32 + u], w_[:], [r_w], [], r_wst[u % 3])
                else:
                    self.dma(pool, w_[:, 0:2048], self.wout[l * 16 + u - 16], [], [r_w], r_w)
                    self.dma(sp, self.wscr[l * 32 + u][:, 0:2048], w_[:, 0:2048], [r_w], [], r_wst[u % 3])
            self.barrier()
            for i_ in range(3):
                h0, r_h0 = hb[i_]
                self.dma(sp, h0[:], srcv[:, :, i_ * 128:(i_ + 1) * 128], [], [r_h0], r_h0)
            ring_prefetch()

            def stageA1_sq(qb):
                h, r_h = hb[qb % 3]
                for c in range(16):
                    I(act, "activation", [r_h], [r_sq16], out=sq16[:, c, :], in_=h[:, c, :], func=AF.Square)

            def stageA1_rest(qb):
                h, r_h = hb[qb % 3]
                stat, r_stat = ps[7]
                for c in range(16):
                    self.mm(stat[:, 0:128], self.onesD[:], sq16[:, c, :], c == 0, c == 15, [r_sq16, self.r_onesD], [r_stat])
                rt_, r_rt = rt
                rs_, r_rs_ = rstd
                I(act, "activation", [r_stat, self.r_epsb], [r_rt], out=rt_[:], in_=stat[:, 0:128], func=AF.Sqrt, bias=self.epsb[:, 0:1], scale=1.0)
                I(dve, "reciprocal", [r_rt], [r_rs_], out=rs_[:], in_=rt_[:])
                for c in range(16):
                    I(dve, "scalar_tensor_tensor", [r_h, r_rs_, self.r_g], [r_xn], out=xn[:, c, :], in0=h[:, c, :],
                      scalar=self.g_sb[:, gcol * 16 + c:gcol * 16 + c + 1], in1=rs_[:], op0=ALU.mult, op1=ALU.mult)

            def stageA(qb):
                tok = slice(qb * 128, (qb + 1) * 128)
                slot3 = qb % 3
                TM, r_TM = ps[6]
                ncol = [512, 512, 512, 512, 512, 256]
                for c in range(16):
                    w_, r_w = ring_pop()
                    for g in range(6):
                        Z_, r_Z = ps[g]
                        self.mm(Z_[:, 0:ncol[g]], xn[:, c, :], w_[:, g * 512:g * 512 + ncol[g]], c == 0, c == 15, [r_xn, r_w], [r_Z])
                    self.mm(TM[:, 0:456], xn[:, c, :], w_[:, 2816:3272], c == 0, c == 15, [r_xn, r_w], [r_TM], inc=True)
                for g in range(6):
                    Z_, r_Z = ps[g]
                    sc = float(64 ** -0.5) if g in (3, 4) else 1.0
                    if g % 2 == 0:
                        I(act, "activation", [r_Z], [r_ztok], out=ztok[:, g * 512:g * 512 + ncol[g]], in_=Z_[:, 0:ncol[g]], func=AF.Copy, scale=sc)
                    else:
                        I(dve, "tensor_scalar", [r_Z], [r_ztok], out=ztok[:, g * 512:g * 512 + ncol[g]], in0=Z_[:, 0:ncol[g]], scalar1=sc, scalar2=None,
                          op0=ALU.mult)
                I(act, "activation", [r_TM], [r_junk, r_st1], out=junk[:, 0:256], in_=TM[:, 0:256], func=AF.Square, accum_out=st1[:, 0:1])
                I(act, "activation", [r_st1, self.r_epsb], [r_st1], out=st1[:, 1:2], in_=st1[:, 0:1], func=AF.Sqrt, bias=self.epsb[:, 0:1], scale=1.0 / 256)
                I(dve, "reciprocal", [r_st1], [r_st1], out=st1[:, 2:3], in_=st1[:, 1:2])
                I(dve, "scalar_tensor_tensor", [r_TM, r_st1, r_kvg], [r_ckv[qb]], out=ckv[:, qb, :], in0=TM[:, 0:256], scalar=st1[:, 2:3],
                  in1=kvg[:], op0=ALU.mult, op1=ALU.mult)
                I(act, "activation", [r_TM], [r_junk, r_st1], out=junk[:, 0:64], in_=TM[:, 256:320], func=AF.Copy, accum_out=st1[:, 3:4])
                I(dve, "tensor_scalar", [r_st1], [r_st1], out=st1[:, 4:5], in0=st1[:, 3:4], scalar1=-1.0 / 64, scalar2=None, op0=ALU.mult)
                I(dve, "tensor_scalar", [r_TM, r_st1], [r_xc], out=xc[:], in0=TM[:, 256:320], scalar1=st1[:, 4:5], scalar2=None, op0=ALU.add)
                I(act, "activation", [r_xc], [r_junk, r_st1], out=junk[:, 0:64], in_=xc[:], func=AF.Square, accum_out=st1[:, 5:6])
                I(act, "activation", [r_st1, self.r_epsb], [r_st1], out=st1[:, 6:7], in_=st1[:, 5:6], func=AF.Sqrt, bias=self.epsb[:, 0:1], scale=1.0 / 64)
                I(dve, "reciprocal", [r_st1], [r_st1], out=st1[:, 7:8], in_=st1[:, 6:7])
                I(dve, "scalar_tensor_tensor", [r_xc, r_st1, r_kig], [r_kn], out=kn[:], in0=xc[:], scalar=st1[:, 7:8], in1=kig[:],
                  op0=ALU.mult, op1=ALU.mult)
                I(dve, "tensor_tensor", [r_kn, r_kib], [r_kiln], out=kiln[:, 0:64], in0=kn[:], in1=kib[:], op=ALU.add)
                I(dve, "tensor_tensor", [r_kn, r_kib], [r_kiln], out=kiln[:, 64:128], in0=kn[:], in1=kib[:], op=ALU.add)
                I(dve, "tensor_scalar", [r_TM], [r_wsc], out=wsc[:], in0=TM[:, 320:328], scalar1=float(512 ** -0.5), scalar2=None, op0=ALU.mult)
                for hh in range(8):
                    I(dve, "tensor_scalar", [r_wsc, self.r_ident], [r_diag], out=diag[:, hh, :], in0=self.ident[:], scalar1=wsc[:, hh:hh + 1],
                      scalar2=None, op0=ALU.mult)
                for k in range(2):
                    for e in range(2):
                        v_, r_v = vpad[slot3][k][e]
                        I(act, "activation", [r_TM], [r_v], out=v_[:, e * 64:(e + 1) * 64], in_=TM[:, 328 + k * 64:328 + (k + 1) * 64], func=AF.Copy)
                qbT, r_qbT = qbTs[qb % 2]
                kb_, r_kb = kbT[slot3]

                def tgroup(chunks, bank, outs):
                    B_, r_B = ps[bank]
                    Bb = B_[:].bitcast(BF16)
                    n = len(chunks)
                    for j, ch in enumerate(chunks):
                        I(pe, "transpose", [r_ztok, self.r_ident], [r_B], out=Bb[:, j * 128:(j + 1) * 128], in_=ztok[:, ch * 128:(ch + 1) * 128],
                          identity=self.ident[:], inc=(j == n - 1))
                    for (eng, dst_ap, r_dst, j0, nj) in outs:
                        src_ap = Bb[:, j0 * 128:(j0 + nj) * 128].rearrange("p (g t) -> p g t", g=nj)
                        if eng is act:
                            I(act, "activation", [r_B], [r_dst], out=dst_ap, in_=src_ap, func=AF.Copy)
                        else:
                            I(dve, "tensor_copy", [r_B], [r_dst], out=dst_ap, in_=src_ap)

                tgroup(list(range(0, 8)), 0, [(act, qaT[:, :, :], r_qaT, 0, 8)])
                tgroup(list(range(8, 12)) + [20, 21], 1, [(dve, qiT[:, :, :], r_qiT, 0, 4), (dve, kb_[:, :, :], r_kb, 4, 2)])
                tgroup(list(range(12, 20)), 2, [(act, qbT[:, :, :], r_qbT, 0, 8)])
                TP, r_TP = ps[5]
                TPb = TP[:].bitcast(BF16)
                for rc in range(2):
                    I(pe, "transpose", [r_ckv[qb], self.r_ident], [r_TP], out=TPb[:, rc * 128:(rc + 1) * 128], in_=ckv[:, qb, rc * 128:(rc + 1) * 128],
                      identity=self.ident[:], inc=False)
                I(pe, "transpose", [r_kiln, self.r_ident], [r_TP], out=TPb[:, 256:384], in_=kiln[:], identity=self.ident[:])
                for rc in range(2):
                    I(act, "activation", [r_TP], [r_ckvT[qb]], out=ckvT[:, rc, tok], in_=TPb[:, rc * 128:(rc + 1) * 128], func=AF.Copy)
                I(act, "activation", [r_TP], [r_kiT[qb]], out=kiT[:, tok], in_=TPb[:, 256:384], func=AF.Copy)

                qlatT, r_qlatT = qlatTs[qb % 2]
                for b4 in range(4):
                    rc = b4 // 2
                    Q_, r_Q = ps[1 + b4]
                    for hi in range(4):
                        hh = (b4 % 2) * 4 + hi
                        self.mm(Q_[:, hi * 128:(hi + 1) * 128], wuk[:, hh, rc * 128:(rc + 1) * 128], qaT[:, hh, :], True, True, [r_wuk, r_qaT], [r_Q],
                                inc=(hi == 3))
                    I(act, "activation", [r_Q], [r_qlatT], out=qlatT[:, rc, (b4 % 2) * 512:(b4 % 2 + 1) * 512], in_=Q_[:], func=AF.Copy,
                      scale=float(128 ** -0.5))

            def stageI(qb):
                if qb < 2:
                    return
                nk = (qb + 1) * 128
                nkg = (nk + 511) // 512
                items = [(kg, hh) for kg in range(nkg) for hh in range(8)]
                scb = [ps[4], ps[7]]

                def emitD(i):
                    kg, hh = items[i]
                    ncols = min(512, nk - kg * 512)
                    kres = [r_kiT[b_] for b_ in range(kg * 4, min(kg * 4 + 4, qb + 1))]
                    c, e = hh // 2, hh % 2
                    Dk, r_Dk = ps[5 + (i % 2)]
                    self.mm(Dk[:, 0:ncols], qiT[e * 64:(e + 1) * 64, c, :], kiT[e * 64:(e + 1) * 64, kg * 512:kg * 512 + ncols], True, True,
                            [r_qiT] + kres, [r_Dk])

                emitD(0)
                for i, (kg, hh) in enumerate(items):
                    if i + 1 < len(items):
                        emitD(i + 1)
                    ncols = min(512, nk - kg * 512)
                    Dk, r_Dk = ps[5 + (i % 2)]
                    rl, r_rl = relu[i % 3]
                    I(act, "activation", [r_Dk], [r_rl], out=rl[:, 0:ncols], in_=Dk[:, 0:ncols], func=AF.Relu)
                    SC, r_SC = scb[kg % 2]
                    self.mm(SC[:, 0:ncols], diag[:, hh, :], rl[:, 0:ncols], hh == 0, hh == 7, [r_diag, r_rl], [r_SC])
                    if hh == 7:
                        if kg == nkg - 1:
                            if ncols > 128:
                                I(dve, "tensor_copy", [r_SC], [r_scores], out=scores[:, kg * 512:kg * 512 + ncols - 128], in_=SC[:, 0:ncols - 128])
                            I(dve, "tensor_tensor", [r_SC, r_caus], [r_scores], out=scores[:, nk - 128:nk], in0=SC[:, ncols - 128:ncols], in1=caus[:],
                              op=ALU.add)
                        else:
                            I(dve, "tensor_copy", [r_SC], [r_scores], out=scores[:, kg * 512:(kg + 1) * 512], in_=SC[:, 0:512])

            def stageB(qb):
                if qb < 2:
                    return
                nk = (qb + 1) * 128
                I(dve, "memset", [], [r_bs], ap=bs[:, 0:1], constant=BIS_LO + BIS_W0 / 2)
                for it in range(NBIS):
                    wk = BIS_W0 / (2 ** (it + 1))
                    wn = BIS_W0 / (2 ** (it + 2))
                    I(dve, "tensor_scalar", [r_scores, r_bs], [r_mask, r_bs], out=mask[:, 0:nk], in0=scores[:, 0:nk], scalar1=bs[:, 0:1],
                      scalar2=None, op0=ALU.is_ge, op1=ALU.add, accum_out=bs[:, 1:2])
                    I(dve, "tensor_scalar", [r_bs], [r_bs], out=bs[:, 2:3], in0=bs[:, 1:2], scalar1=TOPK - 0.5, scalar2=wk, op0=ALU.is_ge,
                      op1=ALU.mult)
                    addc = (wn - wk) if it < NBIS - 1 else (-wk)
                    I(dve, "scalar_tensor_tensor", [r_bs], [r_bs], out=bs[:, 0:1], in0=bs[:, 0:1], scalar=addc, in1=bs[:, 2:3], op0=ALU.add,
                      op1=ALU.add)
                    yield
                I(dve, "tensor_scalar", [r_scores, r_bs], [r_mask], out=mask[:, 0:nk], in0=scores[:, 0:nk], scalar1=bs[:, 0:1], scalar2=None,
                  op0=ALU.is_ge)
                yield

            def stageBT(qb):
                if qb < 2:
                    return
                for g in range((qb + 1 + 3) // 4):
                    MT, r_MT = ps[5 + (g % 2)]
                    MTb = MT[:].bitcast(BF16)
                    n = min(4, qb + 1 - g * 4)
                    for j in range(n):
                        kc = g * 4 + j
                        I(pe, "transpose", [r_mask, self.r_ident], [r_MT], out=MTb[:, j * 128:(j + 1) * 128], in_=mask[:, kc * 128:(kc + 1) * 128],
                          identity=self.ident[:], inc=(j == n - 1))
                    I(act, "activation", [r_MT, r_negb], [r_maskT], out=maskT[:, g * 4:g * 4 + n, :],
                      in_=MTb[:, 0:n * 128].rearrange("p (g t) -> p g t", g=n), func=AF.Identity, scale=-NEG, bias=negb[:, 0:1])

            def stageC(qb, bgen):
                def bstep():
                    if bgen is not None:
                        next(bgen, None)

                tok = slice(qb * 128, (qb + 1) * 128)
                h, r_h = hb[qb % 3]
                use_mask = qb >= 2
                if qb + 2 < NB:
                    stageA1_sq(qb + 2)
                qlatT, r_qlatT = qlatTs[qb % 2]
                qbT, r_qbT = qbTs[qb % 2]
                steps = [(hf, kc) for hf in range(2) for kc in range(qb + 1)]
                accb = [(ps[2], ps[3], ps[4]), (ps[5], ps[6], ps[7])]

                def emitST(i):
                    hf, kc = steps[i]
                    hs = slice(hf * 512, (hf + 1) * 512)
                    ST, r_ST = ps[i % 2]
                    ks = slice(kc * 128, (kc + 1) * 128)
                    hasb = kc >= qb - 1
                    self.mm(ST[:], ckvT[:, 0, ks], qlatT[:, 0, hs], True, False, [r_ckvT[kc], r_qlatT], [r_ST])
                    self.mm(ST[:], ckvT[:, 1, ks], qlatT[:, 1, hs], False, not (hasb or use_mask), [r_ckvT[kc], r_qlatT], [r_ST])
                    if hasb:
                        bi_ = 1 if kc == qb else 0
                        self.mm(ST[:], self.ident[:], self.biasA[:, bi_, hs], False, not use_mask, [self.r_ident, self.r_biasA], [r_ST])
                    if use_mask:
                        self.mm(ST[:].rearrange("p (g t) -> p g t", g=4), self.ident[:],
                                maskT[:, kc, :].unsqueeze(1).to_broadcast([128, 4, 128]), False, True, [self.r_ident, r_maskT], [r_ST])

                emitST(0)
                for i, (hf, kc) in enumerate(steps):
                    if i + 1 < len(steps):
                        emitST(i + 1)
                    hs = slice(hf * 512, (hf + 1) * 512)
                    (ACC0, r_A0), (ACC1, r_A1), (SUMS, r_SU) = accb[hf]
                    ST, r_ST = ps[i % 2]
                    p_, r_p = eT[i % 3]
                    I(act, "activation", [r_ST], [r_p], out=p_[:], in_=ST[:], func=AF.Exp)
                    first, lastk = kc == 0, kc == qb
                    self.mm(ACC0[:], ckv[:, kc, 0:128], p_[:], first, lastk, [r_ckv[kc], r_p], [r_A0], inc=False)
                    self.mm(ACC1[:], ckv[:, kc, 128:256], p_[:], first, lastk, [r_ckv[kc], r_p], [r_A1], inc=False)
                    self.mm(SUMS[:], self.ones[:], p_[:], first, lastk, [self.r_ones, r_p], [r_SU], inc=True)
                    bstep()
                    bstep()
                    if lastk:
                        rs, r_rs = rss[0]
                        I(dve, "reciprocal", [r_SU], [r_rs], out=rs[:], in_=SUMS[:])
                        I(dve, "tensor_tensor", [r_A0, r_rs], [r_olatT], out=olatT[:, 0, hs], in0=ACC0[:], in1=rs[:], op=ALU.mult)
                        I(dve, "tensor_tensor", [r_A1, r_rs], [r_olatT], out=olatT[:, 1, hs], in0=ACC1[:], in1=rs[:], op=ALU.mult)
                chunks = [(qb - 1, 0), (qb, 1)] if qb > 0 else [(qb, 1)]
                sw = [(k, kcb, ci, e) for k in range(2) for (kcb, ci) in chunks for e in range(2)]

                def emitS2(i):
                    k, kcb, ci, e = sw[i]
                    kb2, r_kb2 = kbT[kcb % 3]
                    arr = 0 if k == e else 1
                    ST2, r_ST2 = ps[2 + (i % 2)]
                    self.mm(ST2[:].rearrange("p (g t) -> p g t", g=4), kb2[e * 64:(e + 1) * 64, arr, :], qbT[e * 64:(e + 1) * 64, 4 * k:4 * k + 4, :],
                            True, False, [r_kb2, r_qbT], [r_ST2])
                    off = (k * 2 + e) * 512
                    self.mm(ST2[:], self.ident[:], self.biasB[:, ci, off:off + 512], False, True, [self.r_ident, self.r_biasB], [r_ST2])

                emitS2(0)
                pbs = []
                for i, (k, kcb, ci, e) in enumerate(sw):
                    if i + 1 < len(sw):
                        emitS2(i + 1)
                    ST2, r_ST2 = ps[2 + (i % 2)]
                    pb_, r_pb = pB[i % 4]
                    I(act, "activation", [r_ST2], [r_pb], out=pb_[:], in_=ST2[:], func=AF.Exp)
                    pbs.append((pb_, r_pb, kcb % 3, e))
                    if i + 1 < len(sw) and sw[i + 1][0] == k:
                        continue
                    OB, r_OB = ps[4]
                    SB, r_SB = ps[k]
                    npb = len(pbs)
                    for cp in range(4):
                        for i_, (pb2, r_pb2, kslot, e2) in enumerate(pbs):
                            v_, r_v = vpad[kslot][k][e2]
                            self.mm(OB[:, cp * 128:(cp + 1) * 128], v_[:], pb2[:, cp * 128:(cp + 1) * 128], i_ == 0, i_ == npb - 1, [r_v, r_pb2], [r_OB],
                                    inc=False)
                        for i_, (pb2, r_pb2, kslot, e2) in enumerate(pbs):
                            o_, r_o = opad[e2]
                            self.mm(SB[:, cp * 128:(cp + 1) * 128], o_[:], pb2[:, cp * 128:(cp + 1) * 128], i_ == 0, i_ == npb - 1, [r_o, r_pb2], [r_SB],
                                    inc=(cp == 3 and i_ == npb - 1))
                    den, r_den = dens[0]
                    for cp in range(4):
                        c = 4 * k + cp
                        I(dve, "tensor_scalar", [r_SB, r_esink], [r_den], out=den[:, cp * 128:(cp + 1) * 128], in0=SB[:, cp * 128:(cp + 1) * 128],
                          scalar1=esink[:, c:c + 1], scalar2=None, op0=ALU.add)
                    I(dve, "reciprocal", [r_den], [r_den], out=den[:], in_=den[:])
                    I(dve, "tensor_tensor", [r_OB, r_den], [r_omix], out=omix[:, 8 + 4 * k:8 + 4 * k + 4, :],
                      in0=OB[:].rearrange("p (g t) -> p g t", g=4), in1=den[:].rearrange("p (g t) -> p g t", g=4), op=ALU.mult)
                    pbs = []
                    bstep()
                if bgen is not None:
                    for _ in bgen:
                        pass
                if qb + 1 < NB:
                    stageBT(qb + 1)
                if qb + 2 < NB:
                    stageA1_rest(qb + 2)
                for g in range(2):
                    OA, r_OA = ps[5 + g]
                    for hi in range(4):
                        hh = g * 4 + hi
                        for rc in range(2):
                            self.mm(OA[:, hi * 128:(hi + 1) * 128], wuv[:, rc, hh, :], olatT[:, rc, hh * 128:(hh + 1) * 128], rc == 0, rc == 1,
                                    [r_wuv, r_olatT], [r_OA], inc=(hi == 3 and rc == 1))
                    I(act, "activation", [r_OA], [r_omix], out=omix[:, g * 4:(g + 1) * 4, :], in_=OA[:].rearrange("p (g t) -> p g t", g=4), func=AF.Copy)
                for c in range(16):
                    w_, r_w = ring_pop()
                    for g in range(4):
                        WO, r_WO = ps[g]
                        self.mm(WO[:], omix[:, c, :], w_[:, g * 512:(g + 1) * 512], c == 0, c == 15, [r_omix, r_w], [r_WO], inc=(g == 3 or c == 15))
                for g in range(4):
                    WO, r_WO = ps[g]
                    if g % 2 == 0:
                        I(act, "activation", [r_WO], [r_dtok], out=dtok[:, g * 512:(g + 1) * 512], in_=WO[:], func=AF.Copy)
                    else:
                        I(dve, "tensor_copy", [r_WO], [r_dtok], out=dtok[:, g * 512:(g + 1) * 512], in_=WO[:])
                bstep()
                for g in range(4):
                    TR, r_TR = ps[4 + g]
                    for oi in range(4):
                        o = g * 4 + oi
                        I(pe, "transpose", [r_dtok, r_identf], [r_TR], out=TR[:, oi * 128:(oi + 1) * 128], in_=dtok[:, o * 128:(o + 1) * 128],
                          identity=identf[:], inc=(oi == 3))
                    I(dve, "tensor_tensor", [r_TR, r_h], [r_h], out=h[:, g * 4:(g + 1) * 4, :], in0=TR[:].rearrange("p (g t) -> p g t", g=4),
                      in1=h[:, g * 4:(g + 1) * 4, :], op=ALU.add)
                    bstep()
                self.dma(sp, dstv[:, :, tok], h[:], [r_h], [], r_h)
                if qb + 3 < NB:
                    self.dma(sp, h[:], srcv[:, :, (qb + 3) * 128:(qb + 4) * 128], [], [r_h], r_h)
                if bgen is not None:
                    for _ in bgen:
                        pass

            stageA1_sq(0)
            stageA1_rest(0)
            stageA(0)
            stageI(0)
            stageA1_sq(1)
            stageA1_rest(1)
            for qb in range(NB):
                if qb + 1 < NB:
                    stageA(qb + 1)
                    stageI(qb + 1)
                bgen = stageB(qb + 1) if qb + 1 < NB else None
                stageC(qb, bgen)


def _t5_bucket(dist):
    n = np.maximum(dist, 0)
    nf = np.maximum(n, 1).astype(np.float32)
    large = 16 + (np.log(nf / np.float32(16)) / np.float32(math.log(128 / 16)) * np.float32(16)).astype(np.int32)
    large = np.minimum(large, 31)
    return np.where(n < 16, n, large)


def _consts():
    ident = np.eye(128, dtype=np.float32)
    q = np.arange(128)[:, None]
    j = np.arange(128)[None, :]
    caus = np.where(j > q, NEG, 0.0).astype(np.float32)
    dm = np.eye(32, dtype=np.float32)
    dm[31, :] -= 1.0
    jj = np.arange(128)[:, None]
    ii = np.arange(128)[None, :]
    ea = np.zeros((33, 2, 128, 128), np.float32)
    eb = np.zeros((33, 2, 128, 128), np.float32)
    for ci in range(2):
        dist = (128 + ii - jj) if ci == 0 else (ii - jj)
        bk = _t5_bucket(dist)
        va = dist >= 0
        vb = (dist >= 0) & (dist < 128)
        for b in range(32):
            ea[b, ci] = ((bk == b) & va)
            eb[b, ci] = ((bk == b) & vb)
        ea[32, ci] = ~va
        eb[32, ci] = ~vb
    return ident, caus, dm, ea.reshape(33, -1), eb.reshape(33, -1)


_COLS_FM = (list(range(0, 1024)) + list(range(1280, 1792)) + list(range(1864, 2888)) + list(range(2888, 3016))
            + list(range(2952, 3016)) + list(range(2888, 2952)))
_COLS_TM = list(range(1024, 1280)) + list(range(1792, 1856)) + list(range(1856, 1864)) + list(range(3016, 3144))


def _prep_shared(inp):
    f = lambda a: np.ascontiguousarray(a, dtype=np.float32)
    gl = []
    for l in range(L):
        gl += [inp["ffn1_norm"][l], inp["mix_norm"][l], inp["ffn2_norm"][l]]
    gl.append(inp["final_norm"])
    gains = np.concatenate([np.asarray(g).reshape(16, 128).T for g in gl], axis=1)
    wg, wu, wd = [], [], []
    for l in range(L):
        for (g, u, d_) in ((inp["ffn1_gate"], inp["ffn1_up"], inp["ffn1_down"]), (inp["ffn2_gate"], inp["ffn2_up"], inp["ffn2_down"])):
            wg.append(np.asarray(g[l]).reshape(16, 128, 44, 128).transpose(2, 1, 0, 3).reshape(44, 128, 2048))
            wu.append(np.asarray(u[l]).reshape(16, 128, 44, 128).transpose(2, 1, 0, 3).reshape(44, 128, 2048))
            wd.append(np.asarray(d_[l]).reshape(NFC, 128, 16, 128).transpose(2, 1, 0, 3).reshape(16, 128, NFC * 128))
    winfm, wintm, wout, wuk, wuv, sinkl = [], [], [], [], [], []
    for l in range(L):
        w = np.asarray(inp["w_in"][l])
        winfm.append(w[:, _COLS_FM + _COLS_TM].reshape(16, 128, 3272))
        wout.append(np.asarray(inp["w_out"][l]).reshape(16, 128, 2048))
        wuk.append(np.asarray(inp["w_uk"][l]).transpose(2, 1, 0).reshape(128, 2048))
        wuv.append(np.asarray(inp["w_uv"][l]).reshape(2, 128, 8, 128).transpose(1, 0, 2, 3).reshape(128, 2048))
        s = np.asarray(inp["sinks"][l])
        sl = np.zeros((128, 8), np.float32)
        for c in range(8):
            sl[0:64, c] = s[2 * c]
            sl[64:128, c] = s[2 * c + 1]
        sinkl.append(sl)
    ident, caus, dm, ea, eb = _consts()
    return {
        "gains": f(gains), "wg": f(np.concatenate(wg, 0)), "wu": f(np.concatenate(wu, 0)), "wd": f(np.concatenate(wd, 0)),
        "winr": f(np.concatenate(winfm, 0)), "wout": f(np.concatenate(wout, 0)), "c_identf": f(ident),
        "wuk": f(np.stack(wuk, 0)), "wuv": f(np.stack(wuv, 0)), "kvg": f(inp["kv_norm"]), "kig": f(inp["idx_k_norm_g"]),
        "kib": f(inp["idx_k_norm_b"]), "sinkl": f(np.stack(sinkl, 0)), "relb": f(inp["rel_bias"]),
        "c_ident": f(ident), "c_caus": f(caus), "c_dm": f(dm), "c_ea": f(ea), "c_eb": f(eb),
    }


def kernel(**inputs):
    x = np.asarray(inputs["x"], dtype=np.float32)
    B = x.shape[0]
    shared = _prep_shared(inputs)
    kb = KB()
    nc = kb.build()
    in_maps = []
    for b in range(B):
        m = dict(shared)
        m["xT"] = np.ascontiguousarray(x[b].T)
        in_maps.append(m)
    res = run_bass_kernel_spmd(nc, in_maps, core_ids=list(range(B)))
    out = np.stack([np.ascontiguousarray(r["outT"].T) for r in res.results], axis=0)
    return out.astype(np.float32)
```

```python
import os
import math
from contextlib import ExitStack
import numpy as np
import concourse.bass as bass
import concourse.mybir as mybir
from concourse.bass_utils import run_bass_kernel_spmd

F32 = mybir.dt.float32
BF16 = mybir.dt.bfloat16
ALU = mybir.AluOpType
AF = mybir.ActivationFunctionType

D = 2048
S = 4096
L = 2
DFF = 5632
NFC = DFF // 128
NB = S // 128
EPS = 1e-6
NEG = -30000.0
NBIS = 16
BIS_LO = -16.0
BIS_W0 = 32.0
TOPK = 256


class Res:
    __slots__ = ("name", "w", "r", "dsem")

    def __init__(self, name):
        self.name = name
        self.w = None
        self.r = {}
        self.dsem = None


class Sem:
    def __init__(self, name, sem):
        self.name = name
        self.sem = sem
        self.cnt = 0


class Eng(Sem):
    def __init__(self, name, eng, sem, inorder=False):
        super().__init__(name, sem)
        self.eng = eng
        self.seen = {}
        self.inorder = inorder


class KB:
    def __init__(self, nlayers=L, dbg=None):
        self.nlayers = nlayers
        self.dbg = dbg
        self.nc = bass.Bass("TRN2", target_bir_lowering=False)
        self.es = ExitStack()
        nc = self.nc
        self.nsem = 0
        self.uid = 0
        self.stop_after = None
        self.pe = Eng("pe", nc.tensor, self._sem("pe"), inorder=True)
        self.act = Eng("act", nc.scalar, self._sem("act"))
        self.dve = Eng("dve", nc.vector, self._sem("dve"))
        self.sp = Eng("sp", nc.sync, self._sem("sp"))
        self.pool = Eng("pool", nc.gpsimd, self._sem("pool"))
        self.engs = [self.pe, self.act, self.dve, self.sp, self.pool]
        self.dsems = []

    def _sem(self, name):
        self.nsem += 1
        return self.es.enter_context(self.nc.semaphore(name))

    def sb(self, es, name, shape, dt):
        self.uid += 1
        name = f"{name}_{self.uid}"
        t = es.enter_context(self.nc.sbuf_tensor(name, shape, dt))
        return t, Res(name)

    def dsem_of(self, res):
        if res.dsem is None:
            res.dsem = Sem("d_" + res.name, self._sem("d_" + res.name))
            self.dsems.append(res.dsem)
        return res.dsem

    def _wait(self, E, reads, writes):
        deps = []
        for b in reads:
            if b.w is not None:
                deps.append(b.w)
        for b in writes:
            if b.w is not None:
                deps.append(b.w)
            deps.extend(b.r.values())
        for (X, c) in deps:
            if X is E and E.inorder:
                continue
            if E.seen.get(X.name, 0) >= c:
                continue
            E.eng.wait_ge(X.sem, c)
            E.seen[X.name] = c

    def _mark(self, tok, reads, writes):
        X = tok[0]
        for b in reads:
            b.r[X.name] = tok
        for b in writes:
            b.w = tok
            b.r = {}

    def I(self, E, fn, reads, writes, inc=True, **kw):
        self._wait(E, reads, writes)
        inst = getattr(E.eng, fn)(**kw)
        if inc:
            E.cnt += 1
            inst.then_inc(E.sem, 1)
            tok = (E, E.cnt)
        else:
            tok = (E, E.cnt + 1)
        self._mark(tok, reads, writes)
        return inst

    def dma(self, Q, out, in_, reads, writes, dres):
        ds = self.dsem_of(dres)
        self._wait(Q, reads, writes)
        inst = Q.eng.dma_start(out=out, in_=in_)
        ds.cnt += 16
        inst.then_inc(ds.sem, 16)
        self._mark((ds, ds.cnt), reads, writes)

    def barrier(self):
        allx = self.engs + self.dsems
        for E in self.engs:
            for X in allx:
                if X is E and E.inorder:
                    continue
                if X.cnt > E.seen.get(X.name, 0):
                    E.eng.wait_ge(X.sem, X.cnt)
                    E.seen[X.name] = X.cnt

    def mm(self, out, lhsT, rhs, start, stop, reads, writes, inc=None):
        if inc is None:
            inc = stop
        return self.I(self.pe, "matmul", reads, writes, inc=inc, out=out, lhsT=lhsT, rhs=rhs, start=start, stop=stop)

    def build(self):
        nc = self.nc
        es = self.es
        NL = self.nlayers
        dr = lambda name, shape, kind="ExternalInput": nc.dram_tensor(name, shape, F32, kind=kind).ap()
        self.xT = dr("xT", [D, S])
        self.gains = dr("gains", [128, (3 * L + 1) * 16])
        self.wg = dr("wg", [L * 2 * 44, 128, 16 * 128])
        self.wu = dr("wu", [L * 2 * 44, 128, 16 * 128])
        self.wd = dr("wd", [L * 2 * 16, 128, NFC * 128])
        self.winr = dr("winr", [L * 16, 128, 3272])
        self.wout = dr("wout", [L * 16, 128, 2048])
        self.c_identf = dr("c_identf", [128, 128])
        self.wuk = dr("wuk", [L, 128, 8 * 256])
        self.wuv = dr("wuv", [L, 128, 2 * 8 * 128])
        self.kvg = dr("kvg", [L, 256])
        self.kig = dr("kig", [L, 64])
        self.kib = dr("kib", [L, 64])
        self.sinkl = dr("sinkl", [L, 128, 8])
        self.relb = dr("relb", [32, 24])
        self.c_ident = dr("c_ident", [128, 128])
        self.c_caus = dr("c_caus", [128, 128])
        self.c_dm = dr("c_dm", [32, 32])
        self.c_ea = dr("c_ea", [33, 32768])
        self.c_eb = dr("c_eb", [33, 32768])
        self.outT = dr("outT", [D, S], kind="ExternalOutput")
        skind = "ExternalOutput" if self.dbg else "Internal"
        self.hA = dr("hA", [D, S], kind=skind)
        self.hB = dr("hB", [D, S], kind=skind)
        self.bsA = nc.dram_tensor("bsA", [8, 32768], BF16, kind="Internal").ap()
        self.bsB = nc.dram_tensor("bsB", [16, 32768], BF16, kind="Internal").ap()
        self.wscr = nc.dram_tensor("wscr", [L * 32, 128, 3272], BF16, kind="Internal").ap()

        self.ps = []
        for i in range(8):
            t = es.enter_context(nc.psum_tensor(f"ps{i}", [128, 512], F32))
            self.ps.append((t, Res(f"ps{i}")))
        self.ident, self.r_ident = self.sb(es, "ident", [128, 128], BF16)
        self.onesD, self.r_onesD = self.sb(es, "onesD", [128, 128], BF16)
        self.ones, self.r_ones = self.sb(es, "ones", [128, 128], BF16)
        self.g_sb, self.r_g = self.sb(es, "g_sb", [128, (3 * L + 1) * 16], F32)
        self.biasA, self.r_biasA = self.sb(es, "biasA", [128, 2, 1024], BF16)
        self.biasB, self.r_biasB = self.sb(es, "biasB", [128, 2, 2048], BF16)
        self.dma(self.pool, self.ident[:], self.c_ident[:, :], [], [self.r_ident], self.r_ident)
        self.dma(self.sp, self.g_sb[:], self.gains[:, :], [], [self.r_g], self.r_g)
        self.I(self.dve, "memset", [], [self.r_onesD], ap=self.onesD[:], constant=1.0 / D)
        self.I(self.dve, "memset", [], [self.r_ones], ap=self.ones[:], constant=1.0)

        self.setup_bias()
        self.barrier()

        seq = []
        for l in range(NL):
            seq.append(("ffn", l, 0))
            seq.append(("mix", l))
            seq.append(("ffn", l, 1))
        cur = self.xT
        bufs = [self.hA, self.hB]
        bi = 0
        if self.stop_after is not None:
            seq = seq[:self.stop_after]
        for i, ph in enumerate(seq):
            last = (i == len(seq) - 1) and self.stop_after is None
            dst = self.outT if last else bufs[bi]
            if ph[0] == "ffn":
                self.ffn_phase(cur, dst, ph[1], ph[2], final=last)
            else:
                self.mix_phase(cur, dst, ph[1])
            self.barrier()
            cur = dst
            bi ^= 1
        self.es.close()
        return nc

    def setup_bias(self):
        nc = self.nc
        with ExitStack() as es:
            tbl, r_tbl = self.sb(es, "tbl", [32, 24], F32)
            tblb, r_tblb = self.sb(es, "tblb", [32, 24], BF16)
            dm, r_dm = self.sb(es, "dm", [32, 32], BF16)
            lA, r_lA = self.sb(es, "lA", [33, 8], BF16)
            lB, r_lB = self.sb(es, "lB", [33, 16], BF16)
            E, r_E = self.sb(es, "Eoh", [33, 8192], BF16)
            E2, r_E2 = self.sb(es, "Eoh2", [33, 8192], BF16)
            ob, r_ob = self.sb(es, "obias", [16, 8192], BF16)
            ob2, r_ob2 = self.sb(es, "obias2", [16, 8192], BF16)
            self.dma(self.sp, tbl[:], self.relb[:, :], [], [r_tbl], r_tbl)
            self.dma(self.pool, dm[:], self.c_dm[:, :], [], [r_dm], r_dm)
            self.I(self.dve, "tensor_copy", [r_tbl], [r_tblb], out=tblb[:], in_=tbl[:])
            self.I(self.dve, "memset", [], [r_lA], ap=lA[:], constant=NEG)
            self.I(self.dve, "memset", [], [r_lB], ap=lB[:], constant=NEG)
            p0, r_p0 = self.ps[0]
            self.mm(p0[0:32, 0:8], dm[:], tblb[:, 0:8], True, True, [r_dm, r_tblb], [r_p0])
            self.I(self.dve, "tensor_copy", [r_p0], [r_lA], out=lA[0:32, :], in_=p0[0:32, 0:8])
            self.I(self.dve, "tensor_copy", [r_tblb], [r_lB], out=lB[0:32, :], in_=tblb[:, 8:24])
            for (src, lhs, r_lhs, nh, scr) in ((self.c_ea, lA, r_lA, 8, self.bsA), (self.c_eb, lB, r_lB, 16, self.bsB)):
                for q4 in range(4):
                    Eb, r_Eb = (E, r_E) if q4 % 2 == 0 else (E2, r_E2)
                    o_, r_o = (ob, r_ob) if q4 % 2 == 0 else (ob2, r_ob2)
                    self.dma(self.pool, Eb[:], src[:, q4 * 8192:(q4 + 1) * 8192], [], [r_Eb], r_Eb)
                    for j in range(16):
                        pb, r_pb = self.ps[1 + (j % 2)]
                        self.mm(pb[0:nh, :], lhs[:, 0:nh], Eb[:, j * 512:(j + 1) * 512], True, True, [r_lhs, r_Eb], [r_pb])
                        self.I(self.act, "activation", [r_pb], [r_o], out=o_[0:nh, j * 512:(j + 1) * 512], in_=pb[0:nh, :], func=AF.Copy)
                    self.dma(self.sp, scr[:, q4 * 8192:(q4 + 1) * 8192], o_[0:nh, :], [r_o], [], r_o)
            self.barrier()
            sA = self.bsA.rearrange("h (c j i) -> h c j i", c=2, j=128)
            for ci in range(2):
                for h in range(8):
                    self.dma(self.sp, self.biasA[:, ci, h * 128:(h + 1) * 128], sA[h, ci, :, :], [], [self.r_biasA], self.r_biasA)
            sB = self.bsB.rearrange("h (c j i) -> h c j i", c=2, j=128)
            for ci in range(2):
                for k in range(2):
                    for e in range(2):
                        for cp in range(4):
                            h = 8 * k + 2 * cp + e
                            off = ((k * 2 + e) * 4 + cp) * 128
                            self.dma(self.sp, self.biasB[:, ci, off:off + 128], sB[h, ci, :, :], [], [self.r_biasB], self.r_biasB)
            self.barrier()

    def norm(self, h, r_h, T, gcol, out, r_out, sq, rt, rstd, statbank):
        stat, r_stat = self.ps[statbank]
        for c in range(16):
            s_, r_s = sq[c % 2]
            self.I(self.act, "activation", [r_h], [r_s], out=s_[:, 0:T], in_=h[:, c, :], func=AF.Square)
            self.mm(stat[:, 0:T], self.onesD[:], s_[:, 0:T], c == 0, c == 15, [r_s, self.r_onesD], [r_stat], inc=True)
        rt_, r_rt = rt
        rs_, r_rs = rstd
        self.I(self.act, "activation", [r_stat, self.r_epsb], [r_rt], out=rt_[:, 0:T], in_=stat[:, 0:T], func=AF.Sqrt, bias=self.epsb[:, 0:1], scale=1.0)
        self.I(self.dve, "reciprocal", [r_rt], [r_rs], out=rs_[:, 0:T], in_=rt_[:, 0:T])
        for c in range(16):
            self.I(self.dve, "scalar_tensor_tensor", [r_h, r_rs, self.r_g], [r_out], out=out[:, c, :], in0=h[:, c, :],
                   scalar=self.g_sb[:, gcol * 16 + c:gcol * 16 + c + 1], in1=rs_[:, 0:T], op0=ALU.mult, op1=ALU.mult)

    def ffn_phase(self, src, dst, l, which, final):
        T = 512
        NT = S // T
        srcv = src.rearrange("(c p) t -> p c t", p=128)
        dstv = dst.rearrange("(c p) t -> p c t", p=128)
        fi = l * 2 + which
        gcol = l * 3 + (0 if which == 0 else 2)
        with ExitStack() as es:
            hb = [self.sb(es, f"f_h{i}", [128, 16, T], F32) for i in range(2)]
            xn, r_xn = self.sb(es, "f_xn", [128, 16, T], BF16)
            mid, r_mid = self.sb(es, "f_mid", [128, NFC, T], BF16)
            wgs = [self.sb(es, f"f_wg{i}", [128, 16, 128], BF16) for i in range(2)]
            wus = [self.sb(es, f"f_wu{i}", [128, 16, 128], BF16) for i in range(2)]
            wds = [self.sb(es, f"f_wd{i}", [128, NFC, 128], BF16) for i in range(2)]
            sq = [self.sb(es, f"f_sq{i}", [128, T], BF16) for i in range(2)]
            rt = self.sb(es, "f_rt", [128, T], F32)
            rstd = self.sb(es, "f_rstd", [128, T], F32)
            sg = [self.sb(es, f"f_sg{i}", [128, T], F32) for i in range(2)]
            self.epsb, self.r_epsb = self.sb(es, "f_eps", [128, 1], F32)
            self.I(self.dve, "memset", [], [self.r_epsb], ap=self.epsb[:], constant=EPS)

            units = []
            for t in range(NT):
                for u in range(44):
                    units.append(("gu", u))
                for o in range(16):
                    units.append(("d", o))
            state = {"next": 0}

            def prefetch(upto):
                while state["next"] <= upto and state["next"] < len(units):
                    kind, idx = units[state["next"]]
                    state["next"] += 1
                    slot = idx % 2
                    if kind == "gu":
                        w_, r_w = wgs[slot]
                        self.dma(self.pool, w_[:].rearrange("p c f -> p (c f)"), self.wg[fi * 44 + idx], [], [r_w], r_w)
                        w_, r_w = wus[slot]
                        self.dma(self.pool, w_[:].rearrange("p c f -> p (c f)"), self.wu[fi * 44 + idx], [], [r_w], r_w)
                    else:
                        w_, r_w = wds[slot]
                        self.dma(self.pool, w_[:].rearrange("p c f -> p (c f)"), self.wd[fi * 16 + idx], [], [r_w], r_w)

            h0, r_h0 = hb[0]
            self.dma(self.sp, h0[:], srcv[:, :, 0:T], [], [r_h0], r_h0)
            ui = 0
            prefetch(1)
            for t in range(NT):
                h, r_h = hb[t % 2]
                if t + 1 < NT:
                    hn, r_hn = hb[(t + 1) % 2]
                    self.dma(self.sp, hn[:], srcv[:, :, (t + 1) * T:(t + 2) * T], [], [r_hn], r_hn)
                self.norm(h, r_h, T, gcol, xn, r_xn, sq, rt, rstd, 0)
                for f in range(NFC):
                    prefetch(ui + 1)
                    ui += 1
                    wg_, r_wg = wgs[f % 2]
                    wu_, r_wu = wus[f % 2]
                    G, r_G = self.ps[1 + (f % 2)]
                    U, r_U = self.ps[3 + (f % 2)]
                    for c in range(16):
                        self.mm(G[:], wg_[:, c, :], xn[:, c, :], c == 0, c == 15, [r_wg, r_xn], [r_G])
                    for c in range(16):
                        self.mm(U[:], wu_[:, c, :], xn[:, c, :], c == 0, c == 15, [r_wu, r_xn], [r_U])
                    s_, r_s = sg[f % 2]
                    self.I(self.act, "activation", [r_G], [r_s], out=s_[:], in_=G[:], func=AF.Silu)
                    self.I(self.dve, "tensor_tensor", [r_s, r_U], [r_mid], out=mid[:, f, :], in0=s_[:], in1=U[:], op=ALU.mult)
                for o in range(16):
                    prefetch(ui + 1)
                    ui += 1
                    wd_, r_wd = wds[o % 2]
                    O, r_O = self.ps[5 + (o % 2)]
                    for f in range(NFC):
                        self.mm(O[:], wd_[:, f, :], mid[:, f, :], f == 0, f == NFC - 1, [r_wd, r_mid], [r_O])
                    self.I(self.dve, "scalar_tensor_tensor", [r_O, r_h], [r_h], out=h[:, o, :], in0=O[:], scalar=0.5, in1=h[:, o, :],
                           op0=ALU.mult, op1=ALU.add)
                if final:
                    self.norm(h, r_h, T, 3 * L, h, r_h, sq, rt, rstd, 0)
                    self.dma(self.sp, dstv[:, :, t * T:(t + 1) * T], h[:], [r_h], [], r_h)
                else:
                    self.dma(self.sp, dstv[:, :, t * T:(t + 1) * T], h[:], [r_h], [], r_h)

    def mix_phase(self, src, dst, l):
        nc = self.nc
        srcv = src.rearrange("(c p) t -> p c t", p=128)
        dstv = dst.rearrange("(c p) t -> p c t", p=128)
        gcol = l * 3 + 1
        pe, act, dve, sp, pool = self.pe, self.act, self.dve, self.sp, self.pool
        I = self.I
        with ExitStack() as es:
            sb = lambda name, shape, dt: self.sb(es, name, shape, dt)
            hb = [sb(f"m_h{i}", [128, 16, 128], F32) for i in range(3)]
            xn, r_xn = sb("m_xn", [128, 16, 128], BF16)
            sq16, r_sq16 = sb("m_sq16", [128, 16, 128], BF16)
            rt = sb("m_rt", [128, 128], F32)
            rstd = sb("m_rstd", [128, 128], F32)
            self.epsb, self.r_epsb = sb("m_eps", [128, 1], F32)
            I(dve, "memset", [], [self.r_epsb], ap=self.epsb[:], constant=EPS)
            wuk, r_wuk = sb("m_wuk", [128, 8, 256], BF16)
            wuv, r_wuv = sb("m_wuv", [128, 2, 8, 128], BF16)
            kvg, r_kvg = sb("m_kvg", [128, 256], F32)
            kig, r_kig = sb("m_kig", [128, 64], F32)
            kib, r_kib = sb("m_kib", [128, 64], F32)
            sink, r_sink = sb("m_sink", [128, 8], F32)
            esink, r_esink = sb("m_esink", [128, 8], F32)
            caus, r_caus = sb("m_caus", [128, 128], F32)
            self.dma(pool, wuk[:].rearrange("p c f -> p (c f)"), self.wuk[l], [], [r_wuk], r_wuk)
            self.dma(pool, wuv[:].rearrange("p a c f -> p (a c f)"), self.wuv[l], [], [r_wuv], r_wuv)
            self.dma(sp, kvg[:], self.kvg[l].partition_broadcast(128), [], [r_kvg], r_kvg)
            self.dma(sp, kig[:], self.kig[l].partition_broadcast(128), [], [r_kig], r_kig)
            self.dma(sp, kib[:], self.kib[l].partition_broadcast(128), [], [r_kib], r_kib)
            self.dma(sp, sink[:], self.sinkl[l], [], [r_sink], r_sink)
            self.dma(sp, caus[:], self.c_caus[:, :], [], [r_caus], r_caus)
            I(act, "activation", [r_sink], [r_esink], out=esink[:], in_=sink[:], func=AF.Exp)
            ring = [sb(f"m_ring{i}", [128, 3272], BF16) for i in range(3)]
            ztok, r_ztok = sb("m_ztok", [128, 2816], BF16)
            dtok, r_dtok = sb("m_dtok", [128, 2048], F32)
            identf, r_identf = sb("m_identf", [128, 128], F32)
            self.dma(sp, identf[:], self.c_identf[:, :], [], [r_identf], r_identf)
            ckv, _ = sb("m_ckv", [128, NB, 256], BF16)
            ckvT, _ = sb("m_ckvT", [128, 2, S], BF16)
            kiT, _ = sb("m_kiT", [128, S], BF16)
            r_ckv = [Res(f"ckv{i}") for i in range(NB)]
            r_ckvT = [Res(f"ckvT{i}") for i in range(NB)]
            r_kiT = [Res(f"kiT{i}") for i in range(NB)]
            kbT = [sb(f"m_kbT{i}", [128, 2, 128], BF16) for i in range(3)]
            vpad = [[[sb(f"m_vp{s}{k}{e}", [128, 128], BF16) for e in range(2)] for k in range(2)] for s in range(3)]
            opad = [sb(f"m_op{e}", [128, 128], BF16) for e in range(2)]
            for s_ in range(3):
                for k in range(2):
                    for e in range(2):
                        v_, r_v = vpad[s_][k][e]
                        I(dve, "memset", [], [r_v], ap=v_[:], constant=0.0)
            for e in range(2):
                o_, r_o = opad[e]
                I(dve, "memset", [], [r_o], ap=o_[:], constant=0.0)
                I(dve, "memset", [], [r_o], ap=o_[:, e * 64:(e + 1) * 64], constant=1.0)
            qaT, r_qaT = sb("m_qaT", [128, 8, 128], BF16)
            qiT, r_qiT = sb("m_qiT", [128, 4, 128], BF16)
            qbTs = [sb(f"m_qbT{i}", [128, 8, 128], BF16) for i in range(2)]
            qlatTs = [sb(f"m_qlatT{i}", [128, 2, 1024], BF16) for i in range(2)]
            olatT, r_olatT = sb("m_olatT", [128, 2, 1024], BF16)
            omix, r_omix = sb("m_omix", [128, 16, 128], BF16)
            junk, r_junk = sb("m_junk", [128, 256], F32)
            st1, r_st1 = sb("m_st1", [128, 16], F32)
            xc, r_xc = sb("m_xc", [128, 64], F32)
            kn, r_kn = sb("m_kn", [128, 64], F32)
            kiln, r_kiln = sb("m_kiln", [128, 128], BF16)
            wsc, r_wsc = sb("m_wsc", [128, 8], F32)
            diag, r_diag = sb("m_diag", [128, 8, 128], BF16)
            relu = [sb(f"m_relu{i}", [128, 512], BF16) for i in range(3)]
            scores, r_scores = sb("m_scores", [128, S], F32)
            mask, r_mask = sb("m_mask", [128, S], BF16)
            maskT, r_maskT = sb("m_maskT", [128, NB, 128], BF16)
            bs, r_bs = sb("m_bs", [128, 4], F32)
            eT = [sb(f"m_eT{i}", [128, 512], BF16) for i in range(3)]
            rss = [sb(f"m_rs{i}", [128, 512], F32) for i in range(1)]
            pB = [sb(f"m_pB{i}", [128, 512], BF16) for i in range(4)]
            dens = [sb(f"m_den{i}", [128, 512], F32) for i in range(1)]
            negb, r_negb = sb("m_negb", [128, 1], F32)
            I(dve, "memset", [], [r_negb], ap=negb[:], constant=NEG)

            ps = self.ps
            sched = [("in", 0, i) for i in range(16)]
            for qb in range(NB):
                if qb + 1 < NB:
                    sched += [("in", qb + 1, i) for i in range(16)]
                sched += [("out", qb, i) for i in range(16)]
            rstate = {"issued": 0}
            pending = []

            def ring_prefetch():
                while len(pending) < 2 and rstate["issued"] < len(sched):
                    n = rstate["issued"]
                    rstate["issued"] = n + 1
                    kind, _, i = sched[n]
                    w_, r_w = ring[n % 3]
                    if kind == "in":
                        self.dma(pool, w_[:], self.wscr[l * 32 + i], [], [r_w], r_w)
                    else:
                        self.dma(pool, w_[:, 0:2048], self.wscr[l * 32 + 16 + i][:, 0:2048], [], [r_w], r_w)
                    pending.append((w_, r_w))

            def ring_pop():
                w = pending.pop(0)
                ring_prefetch()
                return w

            r_wst = [Res(f"m_wst{i}_{l}") for i in range(3)]
            for u in range(32):
                w_, r_w = ring[u % 3]
                if u < 16:
                    self.dma(pool, w_[:], self.winr[l * 16 + u], [], [r_w], r_w)
                    self.dma(sp, self.wscr[l * 32 + u], w_[:], [r_w], [], r_wst[u % 3])
                else:
                    self.dma(pool, w_[:, 0:2048], self.wout[l * 16 + u - 16], [], [r_w], r_w)
                    self.dma(sp, self.wscr[l * 32 + u][:, 0:2048], w_[:, 0:2048], [r_w], [], r_wst[u % 3])
            self.barrier()
            for i_ in range(3):
                h0, r_h0 = hb[i_]
                self.dma(sp, h0[:], srcv[:, :, i_ * 128:(i_ + 1) * 128], [], [r_h0], r_h0)
            ring_prefetch()

            def stageA1_sq(qb):
                h, r_h = hb[qb % 3]
                for c in range(16):
                    I(act, "activation", [r_h], [r_sq16], out=sq16[:, c, :], in_=h[:, c, :], func=AF.Square)

            def stageA1_rest(qb):
                h, r_h = hb[qb % 3]
                stat, r_stat = ps[7]
                for c in range(16):
                    self.mm(stat[:, 0:128], self.onesD[:], sq16[:, c, :], c == 0, c == 15, [r_sq16, self.r_onesD], [r_stat])
                rt_, r_rt = rt
                rs_, r_rs_ = rstd
                I(act, "activation", [r_stat, self.r_epsb], [r_rt], out=rt_[:], in_=stat[:, 0:128], func=AF.Sqrt, bias=self.epsb[:, 0:1], scale=1.0)
                I(dve, "reciprocal", [r_rt], [r_rs_], out=rs_[:], in_=rt_[:])
                for c in range(16):
                    I(dve, "scalar_tensor_tensor", [r_h, r_rs_, self.r_g], [r_xn], out=xn[:, c, :], in0=h[:, c, :],
                      scalar=self.g_sb[:, gcol * 16 + c:gcol * 16 + c + 1], in1=rs_[:], op0=ALU.mult, op1=ALU.mult)

            def stageA(qb):
                tok = slice(qb * 128, (qb + 1) * 128)
                slot3 = qb % 3
                TM, r_TM = ps[6]
                ncol = [512, 512, 512, 512, 512, 256]
                for c in range(16):
                    w_, r_w = ring_pop()
                    for g in range(6):
                        Z_, r_Z = ps[g]
                        self.mm(Z_[:, 0:ncol[g]], xn[:, c, :], w_[:, g * 512:g * 512 + ncol[g]], c == 0, c == 15, [r_xn, r_w], [r_Z])
                    self.mm(TM[:, 0:456], xn[:, c, :], w_[:, 2816:3272], c == 0, c == 15, [r_xn, r_w], [r_TM], inc=True)
                for g in range(6):
                    Z_, r_Z = ps[g]
                    sc = float(64 ** -0.5) if g in (3, 4) else 1.0
                    if g % 2 == 0:
                        I(act, "activation", [r_Z], [r_ztok], out=ztok[:, g * 512:g * 512 + ncol[g]], in_=Z_[:, 0:ncol[g]], func=AF.Copy, scale=sc)
                    else:
                        I(dve, "tensor_scalar", [r_Z], [r_ztok], out=ztok[:, g * 512:g * 512 + ncol[g]], in0=Z_[:, 0:ncol[g]], scalar1=sc, scalar2=None,
                          op0=ALU.mult)
                I(act, "activation", [r_TM], [r_junk, r_st1], out=junk[:, 0:256], in_=TM[:, 0:256], func=AF.Square, accum_out=st1[:, 0:1])
                I(act, "activation", [r_st1, self.r_epsb], [r_st1], out=st1[:, 1:2], in_=st1[:, 0:1], func=AF.Sqrt, bias=self.epsb[:, 0:1], scale=1.0 / 256)
                I(dve, "reciprocal", [r_st1], [r_st1], out=st1[:, 2:3], in_=st1[:, 1:2])
                I(dve, "scalar_tensor_tensor", [r_TM, r_st1, r_kvg], [r_ckv[qb]], out=ckv[:, qb, :], in0=TM[:, 0:256], scalar=st1[:, 2:3],
                  in1=kvg[:], op0=ALU.mult, op1=ALU.mult)
                I(act, "activation", [r_TM], [r_junk, r_st1], out=junk[:, 0:64], in_=TM[:, 256:320], func=AF.Copy, accum_out=st1[:, 3:4])
                I(dve, "tensor_scalar", [r_st1], [r_st1], out=st1[:, 4:5], in0=st1[:, 3:4], scalar1=-1.0 / 64, scalar2=None, op0=ALU.mult)
                I(dve, "tensor_scalar", [r_TM, r_st1], [r_xc], out=xc[:], in0=TM[:, 256:320], scalar1=st1[:, 4:5], scalar2=None, op0=ALU.add)
                I(act, "activation", [r_xc], [r_junk, r_st1], out=junk[:, 0:64], in_=xc[:], func=AF.Square, accum_out=st1[:, 5:6])
                I(act, "activation", [r_st1, self.r_epsb], [r_st1], out=st1[:, 6:7], in_=st1[:, 5:6], func=AF.Sqrt, bias=self.epsb[:, 0:1], scale=1.0 / 64)
                I(dve, "reciprocal", [r_st1], [r_st1], out=st1[:, 7:8], in_=st1[:, 6:7])
                I(dve, "scalar_tensor_tensor", [r_xc, r_st1, r_kig], [r_kn], out=kn[:], in0=xc[:], scalar=st1[:, 7:8], in1=kig[:],
                  op0=ALU.mult, op1=ALU.mult)
                I(dve, "tensor_tensor", [r_kn, r_kib], [r_kiln], out=kiln[:, 0:64], in0=kn[:], in1=kib[:], op=ALU.add)
                I(dve, "tensor_tensor", [r_kn, r_kib], [r_kiln], out=kiln[:, 64:128], in0=kn[:], in1=kib[:], op=ALU.add)
                I(dve, "tensor_scalar", [r_TM], [r_wsc], out=wsc[:], in0=TM[:, 320:328], scalar1=float(512 ** -0.5), scalar2=None, op0=ALU.mult)
                for hh in range(8):
                    I(dve, "tensor_scalar", [r_wsc, self.r_ident], [r_diag], out=diag[:, hh, :], in0=self.ident[:], scalar1=wsc[:, hh:hh + 1],
                      scalar2=None, op0=ALU.mult)
                for k in range(2):
                    for e in range(2):
                        v_, r_v = vpad[slot3][k][e]
                        I(act, "activation", [r_TM], [r_v], out=v_[:, e * 64:(e + 1) * 64], in_=TM[:, 328 + k * 64:328 + (k + 1) * 64], func=AF.Copy)
                qbT, r_qbT = qbTs[qb % 2]
                kb_, r_kb = kbT[slot3]

                def tgroup(chunks, bank, outs):
                    B_, r_B = ps[bank]
                    Bb = B_[:].bitcast(BF16)
                    n = len(chunks)
                    for j, ch in enumerate(chunks):
                        I(pe, "transpose", [r_ztok, self.r_ident], [r_B], out=Bb[:, j * 128:(j + 1) * 128], in_=ztok[:, ch * 128:(ch + 1) * 128],
                          identity=self.ident[:], inc=(j == n - 1))
                    for (eng, dst_ap, r_dst, j0, nj) in outs:
                        src_ap = Bb[:, j0 * 128:(j0 + nj) * 128].rearrange("p (g t) -> p g t", g=nj)
                        if eng is act:
                            I(act, "activation", [r_B], [r_dst], out=dst_ap, in_=src_ap, func=AF.Copy)
                        else:
                            I(dve, "tensor_copy", [r_B], [r_dst], out=dst_ap, in_=src_ap)

                tgroup(list(range(0, 8)), 0, [(act, qaT[:, :, :], r_qaT, 0, 8)])
                tgroup(list(range(8, 12)) + [20, 21], 1, [(dve, qiT[:, :, :], r_qiT, 0, 4), (dve, kb_[:, :, :], r_kb, 4, 2)])
                tgroup(list(range(12, 20)), 2, [(act, qbT[:, :, :], r_qbT, 0, 8)])
                TP, r_TP = ps[5]
                TPb = TP[:].bitcast(BF16)
                for rc in range(2):
                    I(pe, "transpose", [r_ckv[qb], self.r_ident], [r_TP], out=TPb[:, rc * 128:(rc + 1) * 128], in_=ckv[:, qb, rc * 128:(rc + 1) * 128],
                      identity=self.ident[:], inc=False)
                I(pe, "transpose", [r_kiln, self.r_ident], [r_TP], out=TPb[:, 256:384], in_=kiln[:], identity=self.ident[:])
                for rc in range(2):
                    I(act, "activation", [r_TP], [r_ckvT[qb]], out=ckvT[:, rc, tok], in_=TPb[:, rc * 128:(rc + 1) * 128], func=AF.Copy)
                I(act, "activation", [r_TP], [r_kiT[qb]], out=kiT[:, tok], in_=TPb[:, 256:384], func=AF.Copy)

                qlatT, r_qlatT = qlatTs[qb % 2]
                for b4 in range(4):
                    rc = b4 // 2
                    Q_, r_Q = ps[1 + b4]
                    for hi in range(4):
                        hh = (b4 % 2) * 4 + hi
                        self.mm(Q_[:, hi * 128:(hi + 1) * 128], wuk[:, hh, rc * 128:(rc + 1) * 128], qaT[:, hh, :], True, True, [r_wuk, r_qaT], [r_Q],
                                inc=(hi == 3))
                    I(act, "activation", [r_Q], [r_qlatT], out=qlatT[:, rc, (b4 % 2) * 512:(b4 % 2 + 1) * 512], in_=Q_[:], func=AF.Copy,
                      scale=float(128 ** -0.5))

            def stageI(qb):
                if qb < 2:
                    return
                nk = (qb + 1) * 128
                nkg = (nk + 511) // 512
                items = [(kg, hh) for kg in range(nkg) for hh in range(8)]
                scb = [ps[4], ps[7]]

                def emitD(i):
                    kg, hh = items[i]
                    ncols = min(512, nk - kg * 512)
                    kres = [r_kiT[b_] for b_ in range(kg * 4, min(kg * 4 + 4, qb + 1))]
                    c, e = hh // 2, hh % 2
                    Dk, r_Dk = ps[5 + (i % 2)]
                    self.mm(Dk[:, 0:ncols], qiT[e * 64:(e + 1) * 64, c, :], kiT[e * 64:(e + 1) * 64, kg * 512:kg * 512 + ncols], True, True,
                            [r_qiT] + kres, [r_Dk])

                emitD(0)
                for i, (kg, hh) in enumerate(items):
                    if i + 1 < len(items):
                        emitD(i + 1)
                    ncols = min(512, nk - kg * 512)
                    Dk, r_Dk = ps[5 + (i % 2)]
                    rl, r_rl = relu[i % 3]
                    I(act, "activation", [r_Dk], [r_rl], out=rl[:, 0:ncols], in_=Dk[:, 0:ncols], func=AF.Relu)
                    SC, r_SC = scb[kg % 2]
                    self.mm(SC[:, 0:ncols], diag[:, hh, :], rl[:, 0:ncols], hh == 0, hh == 7, [r_diag, r_rl], [r_SC])
                    if hh == 7:
                        if kg == nkg - 1:
                            if ncols > 128:
                                I(dve, "tensor_copy", [r_SC], [r_scores], out=scores[:, kg * 512:kg * 512 + ncols - 128], in_=SC[:, 0:ncols - 128])
                            I(dve, "tensor_tensor", [r_SC, r_caus], [r_scores], out=scores[:, nk - 128:nk], in0=SC[:, ncols - 128:ncols], in1=caus[:],
                              op=ALU.add)
                        else:
                            I(dve, "tensor_copy", [r_SC], [r_scores], out=scores[:, kg * 512:(kg + 1) * 512], in_=SC[:, 0:512])

            def stageB(qb):
                if qb < 2:
                    return
                nk = (qb + 1) * 128
                I(dve, "memset", [], [r_bs], ap=bs[:, 0:1], constant=BIS_LO + BIS_W0 / 2)
                for it in range(NBIS):
                    wk = BIS_W0 / (2 ** (it + 1))
                    wn = BIS_W0 / (2 ** (it + 2))
                    I(dve, "tensor_scalar", [r_scores, r_bs], [r_mask, r_bs], out=mask[:, 0:nk], in0=scores[:, 0:nk], scalar1=bs[:, 0:1],
                      scalar2=None, op0=ALU.is_ge, op1=ALU.add, accum_out=bs[:, 1:2])
                    I(dve, "tensor_scalar", [r_bs], [r_bs], out=bs[:, 2:3], in0=bs[:, 1:2], scalar1=TOPK - 0.5, scalar2=wk, op0=ALU.is_ge,
                      op1=ALU.mult)
                    addc = (wn - wk) if it < NBIS - 1 else (-wk)
                    I(dve, "scalar_tensor_tensor", [r_bs], [r_bs], out=bs[:, 0:1], in0=bs[:, 0:1], scalar=addc, in1=bs[:, 2:3], op0=ALU.add,
                      op1=ALU.add)
                    yield
                I(dve, "tensor_scalar", [r_scores, r_bs], [r_mask], out=mask[:, 0:nk], in0=scores[:, 0:nk], scalar1=bs[:, 0:1], scalar2=None,
                  op0=ALU.is_ge)
                yield

            def stageBT(qb):
                if qb < 2:
                    return
                for g in range((qb + 1 + 3) // 4):
                    MT, r_MT = ps[5 + (g % 2)]
                    MTb = MT[:].bitcast(BF16)
                    n = min(4, qb + 1 - g * 4)
                    for j in range(n):
                        kc = g * 4 + j
                        I(pe, "transpose", [r_mask, self.r_ident], [r_MT], out=MTb[:, j * 128:(j + 1) * 128], in_=mask[:, kc * 128:(kc + 1) * 128],
                          identity=self.ident[:], inc=(j == n - 1))
                    I(act, "activation", [r_MT, r_negb], [r_maskT], out=maskT[:, g * 4:g * 4 + n, :],
                      in_=MTb[:, 0:n * 128].rearrange("p (g t) -> p g t", g=n), func=AF.Identity, scale=-NEG, bias=negb[:, 0:1])

            def stageC(qb, bgen):
                def bstep():
                    if bgen is not None:
                        next(bgen, None)

                tok = slice(qb * 128, (qb + 1) * 128)
                h, r_h = hb[qb % 3]
                use_mask = qb >= 2
                if qb + 2 < NB:
                    stageA1_sq(qb + 2)
                qlatT, r_qlatT = qlatTs[qb % 2]
                qbT, r_qbT = qbTs[qb % 2]
                steps = [(hf, kc) for hf in range(2) for kc in range(qb + 1)]
                accb = [(ps[2], ps[3], ps[4]), (ps[5], ps[6], ps[7])]

                def emitST(i):
                    hf, kc = steps[i]
                    hs = slice(hf * 512, (hf + 1) * 512)
                    ST, r_ST = ps[i % 2]
                    ks = slice(kc * 128, (kc + 1) * 128)
                    hasb = kc >= qb - 1
                    self.mm(ST[:], ckvT[:, 0, ks], qlatT[:, 0, hs], True, False, [r_ckvT[kc], r_qlatT], [r_ST])
                    self.mm(ST[:], ckvT[:, 1, ks], qlatT[:, 1, hs], False, not (hasb or use_mask), [r_ckvT[kc], r_qlatT], [r_ST])
                    if hasb:
                        bi_ = 1 if kc == qb else 0
                        self.mm(ST[:], self.ident[:], self.biasA[:, bi_, hs], False, not use_mask, [self.r_ident, self.r_biasA], [r_ST])
                    if use_mask:
                        self.mm(ST[:].rearrange("p (g t) -> p g t", g=4), self.ident[:],
                                maskT[:, kc, :].unsqueeze(1).to_broadcast([128, 4, 128]), False, True, [self.r_ident, r_maskT], [r_ST])

                emitST(0)
                for i, (hf, kc) in enumerate(steps):
                    if i + 1 < len(steps):
                        emitST(i + 1)
                    hs = slice(hf * 512, (hf + 1) * 512)
                    (ACC0, r_A0), (ACC1, r_A1), (SUMS, r_SU) = accb[hf]
                    ST, r_ST = ps[i % 2]
                    p_, r_p = eT[i % 3]
                    I(act, "activation", [r_ST], [r_p], out=p_[:], in_=ST[:], func=AF.Exp)
                    first, lastk = kc == 0, kc == qb
                    self.mm(ACC0[:], ckv[:, kc, 0:128], p_[:], first, lastk, [r_ckv[kc], r_p], [r_A0], inc=False)
                    self.mm(ACC1[:], ckv[:, kc, 128:256], p_[:], first, lastk, [r_ckv[kc], r_p], [r_A1], inc=False)
                    self.mm(SUMS[:], self.ones[:], p_[:], first, lastk, [self.r_ones, r_p], [r_SU], inc=True)
                    bstep()
                    bstep()
                    if lastk:
                        rs, r_rs = rss[0]
                        I(dve, "reciprocal", [r_SU], [r_rs], out=rs[:], in_=SUMS[:])
                        I(dve, "tensor_tensor", [r_A0, r_rs], [r_olatT], out=olatT[:, 0, hs], in0=ACC0[:], in1=rs[:], op=ALU.mult)
                        I(dve, "tensor_tensor", [r_A1, r_rs], [r_olatT], out=olatT[:, 1, hs], in0=ACC1[:], in1=rs[:], op=ALU.mult)
                chunks = [(qb - 1, 0), (qb, 1)] if qb > 0 else [(qb, 1)]
                sw = [(k, kcb, ci, e) for k in range(2) for (kcb, ci) in chunks for e in range(2)]

                def emitS2(i):
                    k, kcb, ci, e = sw[i]
                    kb2, r_kb2 = kbT[kcb % 3]
                    arr = 0 if k == e else 1
                    ST2, r_ST2 = ps[2 + (i % 2)]
                    self.mm(ST2[:].rearrange("p (g t) -> p g t", g=4), kb2[e * 64:(e + 1) * 64, arr, :], qbT[e * 64:(e + 1) * 64, 4 * k:4 * k + 4, :],
                            True, False, [r_kb2, r_qbT], [r_ST2])
                    off = (k * 2 + e) * 512
                    self.mm(ST2[:], self.ident[:], self.biasB[:, ci, off:off + 512], False, True, [self.r_ident, self.r_biasB], [r_ST2])

                emitS2(0)
                pbs = []
                for i, (k, kcb, ci, e) in enumerate(sw):
                    if i + 1 < len(sw):
                        emitS2(i + 1)
                    ST2, r_ST2 = ps[2 + (i % 2)]
                    pb_, r_pb = pB[i % 4]
                    I(act, "activation", [r_ST2], [r_pb], out=pb_[:], in_=ST2[:], func=AF.Exp)
                    pbs.append((pb_, r_pb, kcb % 3, e))
                    if i + 1 < len(sw) and sw[i + 1][0] == k:
                        continue
                    OB, r_OB = ps[4]
                    SB, r_SB = ps[k]
                    npb = len(pbs)
                    for cp in range(4):
                        for i_, (pb2, r_pb2, kslot, e2) in enumerate(pbs):
                            v_, r_v = vpad[kslot][k][e2]
                            self.mm(OB[:, cp * 128:(cp + 1) * 128], v_[:], pb2[:, cp * 128:(cp + 1) * 128], i_ == 0, i_ == npb - 1, [r_v, r_pb2], [r_OB],
                                    inc=False)
                        for i_, (pb2, r_pb2, kslot, e2) in enumerate(pbs):
                            o_, r_o = opad[e2]
                            self.mm(SB[:, cp * 128:(cp + 1) * 128], o_[:], pb2[:, cp * 128:(cp + 1) * 128], i_ == 0, i_ == npb - 1, [r_o, r_pb2], [r_SB],
                                    inc=(cp == 3 and i_ == npb - 1))
                    den, r_den = dens[0]
                    for cp in range(4):
                        c = 4 * k + cp
                        I(dve, "tensor_scalar", [r_SB, r_esink], [r_den], out=den[:, cp * 128:(cp + 1) * 128], in0=SB[:, cp * 128:(cp + 1) * 128],
                          scalar1=esink[:, c:c + 1], scalar2=None, op0=ALU.add)
                    I(dve, "reciprocal", [r_den], [r_den], out=den[:], in_=den[:])
                    I(dve, "tensor_tensor", [r_OB, r_den], [r_omix], out=omix[:, 8 + 4 * k:8 + 4 * k + 4, :],
                      in0=OB[:].rearrange("p (g t) -> p g t", g=4), in1=den[:].rearrange("p (g t) -> p g t", g=4), op=ALU.mult)
                    pbs = []
                    bstep()
                if bgen is not None:
                    for _ in bgen:
                        pass
                if qb + 1 < NB:
                    stageBT(qb + 1)
                if qb + 2 < NB:
                    stageA1_rest(qb + 2)
                for g in range(2):
                    OA, r_OA = ps[5 + g]
                    for hi in range(4):
                        hh = g * 4 + hi
                        for rc in range(2):
                            self.mm(OA[:, hi * 128:(hi + 1) * 128], wuv[:, rc, hh, :], olatT[:, rc, hh * 128:(hh + 1) * 128], rc == 0, rc == 1,
                                    [r_wuv, r_olatT], [r_OA], inc=(hi == 3 and rc == 1))
                    I(act, "activation", [r_OA], [r_omix], out=omix[:, g * 4:(g + 1) * 4, :], in_=OA[:].rearrange("p (g t) -> p g t", g=4), func=AF.Copy)
                for c in range(16):
                    w_, r_w = ring_pop()
                    for g in range(4):
                        WO, r_WO = ps[g]
                        self.mm(WO[:], omix[:, c, :], w_[:, g * 512:(g + 1) * 512], c == 0, c == 15, [r_omix, r_w], [r_WO], inc=(g == 3 or c == 15))
                for g in range(4):
                    WO, r_WO = ps[g]
                    if g % 2 == 0:
                        I(act, "activation", [r_WO], [r_dtok], out=dtok[:, g * 512:(g + 1) * 512], in_=WO[:], func=AF.Copy)
                    else:
                        I(dve, "tensor_copy", [r_WO], [r_dtok], out=dtok[:, g * 512:(g + 1) * 512], in_=WO[:])
                bstep()
                for g in range(4):
                    TR, r_TR = ps[4 + g]
                    for oi in range(4):
                        o = g * 4 + oi
                        I(pe, "transpose", [r_dtok, r_identf], [r_TR], out=TR[:, oi * 128:(oi + 1) * 128], in_=dtok[:, o * 128:(o + 1) * 128],
                          identity=identf[:], inc=(oi == 3))
                    I(dve, "tensor_tensor", [r_TR, r_h], [r_h], out=h[:, g * 4:(g + 1) * 4, :], in0=TR[:].rearrange("p (g t) -> p g t", g=4),
                      in1=h[:, g * 4:(g + 1) * 4, :], op=ALU.add)
                    bstep()
                self.dma(sp, dstv[:, :, tok], h[:], [r_h], [], r_h)
                if qb + 3 < NB:
                    self.dma(sp, h[:], srcv[:, :, (qb + 3) * 128:(qb + 4) * 128], [], [r_h], r_h)
                if bgen is not None:
                    for _ in bgen:
                        pass

            stageA1_sq(0)
            stageA1_rest(0)
            stageA(0)
            stageI(0)
            stageA1_sq(1)
            stageA1_rest(1)
            for qb in range(NB):
                if qb + 1 < NB:
                    stageA(qb + 1)
                    stageI(qb + 1)
                bgen = stageB(qb + 1) if qb + 1 < NB else None
                stageC(qb, bgen)


def _t5_bucket(dist):
    n = np.maximum(dist, 0)
    nf = np.maximum(n, 1).astype(np.float32)
    large = 16 + (np.log(nf / np.float32(16)) / np.float32(math.log(128 / 16)) * np.float32(16)).astype(np.int32)
    large = np.minimum(large, 31)
    return np.where(n < 16, n, large)


def _consts():
    ident = np.eye(128, dtype=np.float32)
    q = np.arange(128)[:, None]
    j = np.arange(128)[None, :]
    caus = np.where(j > q, NEG, 0.0).astype(np.float32)
    dm = np.eye(32, dtype=np.float32)
    dm[31, :] -= 1.0
    jj = np.arange(128)[:, None]
    ii = np.arange(128)[None, :]
    ea = np.zeros((33, 2, 128, 128), np.float32)
    eb = np.zeros((33, 2, 128, 128), np.float32)
    for ci in range(2):
        dist = (128 + ii - jj) if ci == 0 else (ii - jj)
        bk = _t5_bucket(dist)
        va = dist >= 0
        vb = (dist >= 0) & (dist < 128)
        for b in range(32):
            ea[b, ci] = ((bk == b) & va)
            eb[b, ci] = ((bk == b) & vb)
        ea[32, ci] = ~va
        eb[32, ci] = ~vb
    return ident, caus, dm, ea.reshape(33, -1), eb.reshape(33, -1)


_COLS_FM = (list(range(0, 1024)) + list(range(1280, 1792)) + list(range(1864, 2888)) + list(range(2888, 3016))
            + list(range(2952, 3016)) + list(range(2888, 2952)))
_COLS_TM = list(range(1024, 1280)) + list(range(1792, 1856)) + list(range(1856, 1864)) + list(range(3016, 3144))


def _prep_shared(inp):
    f = lambda a: np.ascontiguousarray(a, dtype=np.float32)
    gl = []
    for l in range(L):
        gl += [inp["ffn1_norm"][l], inp["mix_norm"][l], inp["ffn2_norm"][l]]
    gl.append(inp["final_norm"])
    gains = np.concatenate([np.asarray(g).reshape(16, 128).T for g in gl], axis=1)
    wg, wu, wd = [], [], []
    for l in range(L):
        for (g, u, d_) in ((inp["ffn1_gate"], inp["ffn1_up"], inp["ffn1_down"]), (inp["ffn2_gate"], inp["ffn2_up"], inp["ffn2_down"])):
            wg.append(np.asarray(g[l]).reshape(16, 128, 44, 128).transpose(2, 1, 0, 3).reshape(44, 128, 2048))
            wu.append(np.asarray(u[l]).reshape(16, 128, 44, 128).transpose(2, 1, 0, 3).reshape(44, 128, 2048))
            wd.append(np.asarray(d_[l]).reshape(NFC, 128, 16, 128).transpose(2, 1, 0, 3).reshape(16, 128, NFC * 128))
    winfm, wintm, wout, wuk, wuv, sinkl = [], [], [], [], [], []
    for l in range(L):
        w = np.asarray(inp["w_in"][l])
        winfm.append(w[:, _COLS_FM + _COLS_TM].reshape(16, 128, 3272))
        wout.append(np.asarray(inp["w_out"][l]).reshape(16, 128, 2048))
        wuk.append(np.asarray(inp["w_uk"][l]).transpose(2, 1, 0).reshape(128, 2048))
        wuv.append(np.asarray(inp["w_uv"][l]).reshape(2, 128, 8, 128).transpose(1, 0, 2, 3).reshape(128, 2048))
        s = np.asarray(inp["sinks"][l])
        sl = np.zeros((128, 8), np.float32)
        for c in range(8):
            sl[0:64, c] = s[2 * c]
            sl[64:128, c] = s[2 * c + 1]
        sinkl.append(sl)
    ident, caus, dm, ea, eb = _consts()
    return {
        "gains": f(gains), "wg": f(np.concatenate(wg, 0)), "wu": f(np.concatenate(wu, 0)), "wd": f(np.concatenate(wd, 0)),
        "winr": f(np.concatenate(winfm, 0)), "wout": f(np.concatenate(wout, 0)), "c_identf": f(ident),
        "wuk": f(np.stack(wuk, 0)), "wuv": f(np.stack(wuv, 0)), "kvg": f(inp["kv_norm"]), "kig": f(inp["idx_k_norm_g"]),
        "kib": f(inp["idx_k_norm_b"]), "sinkl": f(np.stack(sinkl, 0)), "relb": f(inp["rel_bias"]),
        "c_ident": f(ident), "c_caus": f(caus), "c_dm": f(dm), "c_ea": f(ea), "c_eb": f(eb),
    }


def kernel(**inputs):
    x = np.asarray(inputs["x"], dtype=np.float32)
    B = x.shape[0]
    shared = _prep_shared(inputs)
    kb = KB()
    nc = kb.build()
    in_maps = []
    for b in range(B):
        m = dict(shared)
        m["xT"] = np.ascontiguousarray(x[b].T)
        in_maps.append(m)
    res = run_bass_kernel_spmd(nc, in_maps, core_ids=list(range(B)))
    out = np.stack([np.ascontiguousarray(r["outT"].T) for r in res.results], axis=0)
    return out.astype(np.float32)
```
